# Optimizing a Trainium2 kernel written in Bass

```python
import math
import jax, jax.numpy as jnp
from jax import lax
import numpy as np

D_MODEL = 1024
BATCH = 16
SEQ = 4096
DEPTH = 1

HEAD_DIM = 64
NSA_KV_GROUPS = 2
NSA_HEADS = (D_MODEL // 2) // HEAD_DIM
NSA_HPG = NSA_HEADS // NSA_KV_GROUPS
NSA_WIDTH = NSA_HEADS * HEAD_DIM
NSA_BRANCHES = 3
CMP_BLOCK = 32
CMP_STRIDE = 16
CMP_HIDDEN = 256
SEL_BLOCK = 64
SEL_TOPN = 16
WINDOW = 512
DIFF_QK_DIM = 64
DIFF_V_DIM = 2 * DIFF_QK_DIM
DIFF_HEADS = (D_MODEL - NSA_WIDTH) // DIFF_V_DIM
DIFF_WIDTH = DIFF_HEADS * DIFF_V_DIM
MIX_WIDTH = NSA_WIDTH + DIFF_WIDTH
N_ATTN_HEADS = NSA_HEADS + DIFF_HEADS
OFF_NSA_KV = NSA_WIDTH
OFF_NSA_GATE = OFF_NSA_KV + NSA_BRANCHES * 2 * NSA_KV_GROUPS * HEAD_DIM
OFF_DIFF_Q = OFF_NSA_GATE + NSA_HEADS * NSA_BRANCHES
OFF_DIFF_K = OFF_DIFF_Q + DIFF_HEADS * 2 * DIFF_QK_DIM
OFF_DIFF_V = OFF_DIFF_K + DIFF_HEADS * 2 * DIFF_QK_DIM
IN_COLS = OFF_DIFF_V + DIFF_WIDTH
REL_BUCKETS = 32
REL_MAX_DIST = 128
PEER_HEADS = 8
PEER_NKEYS = 128
PEER_EXPERTS = PEER_NKEYS ** 2
PEER_TOPK = 16
PEER_QDIM = 256
QBLOCK = 128
PEER_CHUNK = 128
RMS_EPS = 1e-6
NEG_INF = -1e30
FORCED_SCORE = 1e9

kernel_name = 'hybrid_nsa_diffattn_peer_block'


def rmsnorm(x, g):
    xf = x.astype(jnp.float32)
    y = xf * lax.rsqrt(jnp.mean(xf * xf, axis=-1, keepdims=True) + RMS_EPS)
    return (y * g.astype(jnp.float32)).astype(x.dtype)


def t5_bucket(dist):
    n = jnp.maximum(dist, 0)
    max_exact = REL_BUCKETS // 2
    nf = jnp.maximum(n, 1).astype(jnp.float32)
    large = max_exact + (jnp.log(nf / max_exact) / math.log(REL_MAX_DIST / max_exact)
                         * (REL_BUCKETS - max_exact)).astype(jnp.int32)
    return jnp.where(n < max_exact, n, jnp.minimum(large, REL_BUCKETS - 1))


def masked_softmax(logits, mask):
    p = jax.nn.softmax(jnp.where(mask, logits, NEG_INF), axis=-1)
    return p * mask.astype(p.dtype)


def compress_blocks(tok, pos_enc, w1, w2, starts):
    idx = starts[:, None] + np.arange(CMP_BLOCK)[None, :]
    blocks = tok[:, idx] + pos_enc[None, None, :, None, :].astype(tok.dtype)
    b, nc = blocks.shape[:2]
    flat = blocks.transpose(0, 1, 3, 2, 4).reshape(b, nc, NSA_KV_GROUPS, CMP_BLOCK * HEAD_DIM)
    return jax.nn.gelu(flat @ w1) @ w2


def block_overlap(cmp_start, n_sel):
    sel_start = np.arange(n_sel) * SEL_BLOCK
    lo = np.maximum(cmp_start[:, None], sel_start[None, :])
    hi = np.minimum(cmp_start[:, None] + CMP_BLOCK, sel_start[None, :] + SEL_BLOCK)
    return np.maximum(hi - lo, 0).astype(np.float32) / CMP_BLOCK


def nsa_diff_mixer(h, w_in, w_out, rel_bias, cmp_pos, cmp_w1, cmp_w2,
                   lam_q1, lam_k1, lam_q2, lam_k2, subln_g, layer_idx):
    bsz, s, _ = h.shape
    dt = h.dtype
    f32 = jnp.float32
    G, HPG, DH = NSA_KV_GROUPS, NSA_HPG, HEAD_DIM
    proj = h @ w_in
    q_nsa = proj[..., :OFF_NSA_KV].reshape(bsz, s, G, HPG, DH)
    kv = proj[..., OFF_NSA_KV:OFF_NSA_GATE].reshape(bsz, s, NSA_BRANCHES, 2, G, DH)
    gates = jax.nn.sigmoid(proj[..., OFF_NSA_GATE:OFF_DIFF_Q]).reshape(bsz, s, G, HPG, NSA_BRANCHES)
    dq = proj[..., OFF_DIFF_Q:OFF_DIFF_K].reshape(bsz, s, DIFF_HEADS, 2, DIFF_QK_DIM)
    dk = proj[..., OFF_DIFF_K:OFF_DIFF_V].reshape(bsz, s, DIFF_HEADS, 2, DIFF_QK_DIM)
    dv = proj[..., OFF_DIFF_V:].reshape(bsz, s, DIFF_HEADS, DIFF_V_DIM)

    n_cmp = (s - CMP_BLOCK) // CMP_STRIDE + 1
    cmp_start = np.arange(n_cmp) * CMP_STRIDE
    k_cmp = compress_blocks(kv[:, :, 0, 0], cmp_pos[0], cmp_w1[0], cmp_w2[0], cmp_start)
    v_cmp = compress_blocks(kv[:, :, 0, 1], cmp_pos[1], cmp_w1[1], cmp_w2[1], cmp_start)
    cmp_end = jnp.asarray(cmp_start + CMP_BLOCK - 1, jnp.int32)
    n_sel = s // SEL_BLOCK
    n_top = min(SEL_TOPN, n_sel)
    cmp_to_sel = jnp.asarray(block_overlap(cmp_start, n_sel))
    k_sel = kv[:, :, 1, 0].reshape(bsz, n_sel, SEL_BLOCK, G, DH).transpose(0, 3, 1, 2, 4)
    v_sel = kv[:, :, 1, 1].reshape(bsz, n_sel, SEL_BLOCK, G, DH).transpose(0, 3, 1, 2, 4)
    pad = ((0, 0), (WINDOW, 0), (0, 0), (0, 0))
    k_win = jnp.pad(kv[:, :, 2, 0], pad)
    v_win = jnp.pad(kv[:, :, 2, 1], pad)

    tab_nsa = rel_bias[:, :NSA_HEADS].astype(f32).reshape(REL_BUCKETS, G, HPG)
    tab_nsa_g = tab_nsa.transpose(1, 0, 2)
    tab_diff = rel_bias[:, NSA_HEADS:].astype(f32)

    lam_init = 0.8 - 0.6 * math.exp(-0.3 * layer_idx)
    lam = (jnp.exp(jnp.sum(lam_q1.astype(f32) * lam_k1.astype(f32)))
           - jnp.exp(jnp.sum(lam_q2.astype(f32) * lam_k2.astype(f32))) + lam_init)

    scale = HEAD_DIM ** -0.5
    scale_d = DIFF_QK_DIM ** -0.5
    n_qb = s // QBLOCK
    q_offs = jnp.arange(QBLOCK)
    tok_offs = jnp.arange(SEL_BLOCK)
    blk_ids = jnp.arange(n_sel)
    win_offs = jnp.arange(WINDOW + QBLOCK)
    key_pos = jnp.arange(s)
    g_ids = jnp.arange(G)[:, None, None]

    def rows(a, b, start, size):
        return lax.dynamic_slice(a, (b, start) + (0,) * (a.ndim - 2), (1, size) + a.shape[2:])[0]

    def batch(a, b):
        return lax.dynamic_index_in_dim(a, b, 0, keepdims=False)

    def query_block(i):
        b = i // n_qb
        q0 = (i % n_qb) * QBLOCK
        t = q0 + q_offs
        q = rows(q_nsa, b, q0, QBLOCK)
        gt = rows(gates, b, q0, QBLOCK)

        d_c = t[:, None] - cmp_end[None, :]
        lg_c = (jnp.einsum('tghd,cgd->ghtc', q, batch(k_cmp, b)).astype(f32) * scale
                + tab_nsa[t5_bucket(d_c)].transpose(2, 3, 0, 1))
        p_c = masked_softmax(lg_c, d_c >= 0)
        o_c = jnp.einsum('ghtc,cgd->tghd', p_c.astype(dt), batch(v_cmp, b))

        imp = jnp.einsum('ghtc,cj->gtj', p_c, cmp_to_sel)
        cur = (t // SEL_BLOCK)[:, None]
        forced = (blk_ids == 0) | (blk_ids == cur) | (blk_ids == cur - 1)
        score = jnp.where(forced, FORCED_SCORE, jnp.where(blk_ids <= cur, imp, NEG_INF))
        _, sel = lax.top_k(score, n_top)
        sel_pos = sel[..., None] * SEL_BLOCK + tok_offs
        d_s = t[None, :, None, None] - sel_pos
        nk = n_top * SEL_BLOCK
        ks_g = batch(k_sel, b)[g_ids, sel].reshape(G, QBLOCK, nk, DH)
        vs_g = batch(v_sel, b)[g_ids, sel].reshape(G, QBLOCK, nk, DH)
        b_s = jnp.moveaxis(tab_nsa_g[g_ids[..., None], t5_bucket(d_s)], -1, 1).reshape(G, HPG, QBLOCK, nk)
        lg_s = jnp.einsum('tghd,gtkd->ghtk', q, ks_g).astype(f32) * scale + b_s
        p_s = masked_softmax(lg_s, (d_s >= 0).reshape(G, 1, QBLOCK, nk))
        o_s = jnp.einsum('ghtk,gtkd->tghd', p_s.astype(dt), vs_g)

        kp = q0 - WINDOW + win_offs
        d_w = t[:, None] - kp[None, :]
        ok_w = (d_w >= 0) & (d_w < WINDOW) & (kp >= 0)[None, :]
        lg_w = (jnp.einsum('tghd,sgd->ghts', q, rows(k_win, b, q0, WINDOW + QBLOCK)).astype(f32) * scale
                + tab_nsa[t5_bucket(d_w)].transpose(2, 3, 0, 1))
        p_w = masked_softmax(lg_w, ok_w)
        o_w = jnp.einsum('ghts,sgd->tghd', p_w.astype(dt), rows(v_win, b, q0, WINDOW + QBLOCK))

        o_nsa = (gt[..., 0:1] * o_c + gt[..., 1:2] * o_s + gt[..., 2:3] * o_w).reshape(QBLOCK, NSA_WIDTH)

        qd = rows(dq, b, q0, QBLOCK)
        d_d = t[:, None] - key_pos[None, :]
        lg_d = (jnp.einsum('thmd,shmd->mhts', qd, batch(dk, b)).astype(f32) * scale_d
                + tab_diff[t5_bucket(d_d)].transpose(2, 0, 1)[None])
        p_d = masked_softmax(lg_d, d_d >= 0)
        o_d = jnp.einsum('hts,shd->thd', (p_d[0] - lam * p_d[1]).astype(dt), batch(dv, b))
        o_d = rmsnorm(o_d, subln_g) * (1.0 - lam_init)
        return jnp.concatenate([o_nsa, o_d.reshape(QBLOCK, DIFF_WIDTH)], axis=-1)

    mixed = lax.map(query_block, jnp.arange(bsz * n_qb)).reshape(bsz, s, MIX_WIDTH)
    return mixed @ w_out


def peer_ffn(h, wq, sub_keys, u, v):
    bsz, s, d = h.shape
    dt = h.dtype
    chunks = h.reshape(bsz * s // PEER_CHUNK, PEER_CHUNK, d)
    kk = PEER_TOPK * PEER_TOPK

    def body(xc):
        q = (xc @ wq).reshape(PEER_CHUNK, PEER_HEADS, 2, PEER_QDIM // 2)
        sub = jnp.einsum('thpd,pkd->thpk', q, sub_keys).astype(jnp.float32)
        sv, si = lax.top_k(sub, PEER_TOPK)
        cand = (sv[:, :, 0, :, None] + sv[:, :, 1, None, :]).reshape(PEER_CHUNK, PEER_HEADS, kk)
        cidx = (si[:, :, 0, :, None] * PEER_NKEYS + si[:, :, 1, None, :]).reshape(PEER_CHUNK, PEER_HEADS, kk)
        best, pos = lax.top_k(cand, PEER_TOPK)
        eid = jnp.take_along_axis(cidx, pos, axis=-1)
        g = jax.nn.softmax(best, axis=-1).astype(dt)
        act = jax.nn.gelu(jnp.einsum('td,thkd->thk', xc, u[eid]))
        return jnp.einsum('thk,thkd->td', g * act, v[eid])

    return lax.map(body, chunks).reshape(bsz, s, d)


def setup_inputs(seed: int = 0) -> dict:
    key = jax.random.key(seed)
    ks = jax.random.split(key, 22)
    f32 = jnp.float32
    L, D = DEPTH, D_MODEL

    def nrm(k, shape, sd):
        return sd * jax.random.normal(k, shape, f32)

    return {
        'x': nrm(ks[0], (BATCH, SEQ, D), 1.0),
        'c': nrm(ks[1], (BATCH, D), 1.0),
        'rel_bias': nrm(ks[2], (REL_BUCKETS, N_ATTN_HEADS), 0.5),
        'ada_w': nrm(ks[3], (L, D, 6 * D), 0.5 * D ** -0.5),
        'ada_b': nrm(ks[4], (L, 6 * D), 0.02),
        'norm1_g': 1.0 + nrm(ks[5], (L, D), 0.02),
        'norm2_g': 1.0 + nrm(ks[6], (L, D), 0.02),
        'w_in': nrm(ks[7], (L, D, IN_COLS), D ** -0.5),
        'w_out': nrm(ks[8], (L, MIX_WIDTH, D), MIX_WIDTH ** -0.5),
        'cmp_pos': nrm(ks[9], (L, 2, CMP_BLOCK, HEAD_DIM), 0.02),
        'cmp_w1': nrm(ks[10], (L, 2, CMP_BLOCK * HEAD_DIM, CMP_HIDDEN), (CMP_BLOCK * HEAD_DIM) ** -0.5),
        'cmp_w2': nrm(ks[11], (L, 2, CMP_HIDDEN, HEAD_DIM), CMP_HIDDEN ** -0.5),
        'lam_q1': nrm(ks[12], (L, DIFF_QK_DIM), 0.1),
        'lam_k1': nrm(ks[13], (L, DIFF_QK_DIM), 0.1),
        'lam_q2': nrm(ks[14], (L, DIFF_QK_DIM), 0.1),
        'lam_k2': nrm(ks[15], (L, DIFF_QK_DIM), 0.1),
        'diff_subln_g': 1.0 + nrm(ks[16], (L, DIFF_V_DIM), 0.02),
        'peer_wq': nrm(ks[17], (L, D, PEER_HEADS * PEER_QDIM), D ** -0.5),
        'peer_sub_keys': nrm(ks[18], (L, 2, PEER_NKEYS, PEER_QDIM // 2), (PEER_QDIM // 2) ** -0.5),
        'peer_u': nrm(ks[19], (L, PEER_EXPERTS, D), D ** -0.5),
        'peer_v': nrm(ks[20], (L, PEER_EXPERTS, D), 1.0),
        'final_g': 1.0 + nrm(ks[21], (D,), 0.02),
    }


def reference(x, c, rel_bias, ada_w, ada_b, norm1_g, norm2_g, w_in, w_out, cmp_pos, cmp_w1, cmp_w2,
              lam_q1, lam_k1, lam_q2, lam_k2, diff_subln_g, peer_wq, peer_sub_keys, peer_u, peer_v, final_g):
    cond = jax.nn.silu(c)
    for l in range(DEPTH):
        mod = (cond @ ada_w[l] + ada_b[l]).reshape(c.shape[0], 6, 1, D_MODEL)
        sh_a, sc_a, g_a, sh_f, sc_f, g_f = (mod[:, i] for i in range(6))
        h = rmsnorm(x, norm1_g[l]) * (1 + sc_a) + sh_a
        x = x + g_a * nsa_diff_mixer(h, w_in[l], w_out[l], rel_bias, cmp_pos[l], cmp_w1[l], cmp_w2[l],
                                     lam_q1[l], lam_k1[l], lam_q2[l], lam_k2[l], diff_subln_g[l], l)
        h = rmsnorm(x, norm2_g[l]) * (1 + sc_f) + sh_f
        x = x + g_f * peer_ffn(h, peer_wq[l], peer_sub_keys[l], peer_u[l], peer_v[l])
    return rmsnorm(x, final_g)
```

```python
import math
from contextlib import ExitStack
import numpy as np
import concourse.bass as bass
import concourse.mybir as mybir
from concourse.bass_utils import run_bass_kernel_spmd

F32 = mybir.dt.float32
BF = mybir.dt.bfloat16
ALU = mybir.AluOpType
AF = mybir.ActivationFunctionType
AX = mybir.AxisListType

D = 1024
NEG = -30000.0
EPOCH = 16000
NDMA_SEMS = 24


class Planner:
    ENGS = ("pe", "act", "dve", "pool", "sp")

    def __init__(self, nc, stack):
        self.nc = nc
        self.stack = stack
        self.items = {e: [] for e in self.ENGS}
        self.seq = {e: 0 for e in self.ENGS}
        self.esems = {e: [] for e in self.ENGS}
        self.waited = {e: {} for e in self.ENGS}
        self.semobjs = []
        self.last_w = {}
        self.readers = {}
        self.dma_sems = []
        self.dma_cum = []
        self.dma_next = 0
        self.last_ev = {}

    def _newsem(self, name):
        s = self.stack.enter_context(self.nc.semaphore(name))
        self.semobjs.append(s)
        return len(self.semobjs) - 1

    def _eng_event(self, e):
        n = self.seq[e]
        self.seq[e] += 1
        ep = n // EPOCH
        while len(self.esems[e]) <= ep:
            self.esems[e].append(self._newsem(f"s_{e}_{len(self.esems[e])}"))
        return (self.esems[e][ep], n % EPOCH + 1)

    def _dma_event(self, extra):
        if len(self.dma_sems) < NDMA_SEMS:
            self.dma_sems.append(self._newsem(f"s_dma_{len(self.dma_sems)}"))
            self.dma_cum.append(0)
        i = self.dma_next % NDMA_SEMS
        self.dma_next += 1
        if self.dma_cum[i] > 0:
            extra.append((self.dma_sems[i], self.dma_cum[i]))
        self.dma_cum[i] += 16
        return (self.dma_sems[i], self.dma_cum[i])

    def _deps(self, reads, writes):
        evs = []
        for k in reads:
            w = self.last_w.get(k)
            if w is not None:
                evs.append(w)
        for k in writes:
            w = self.last_w.get(k)
            if w is not None:
                evs.append(w)
            evs.extend(self.readers.get(k, ()))
        return evs

    def _commit(self, ev, reads, writes):
        for k in reads:
            self.readers.setdefault(k, []).append(ev)
        for k in writes:
            self.last_w[k] = ev
            self.readers[k] = []

    def _filter(self, e, evs):
        best = {}
        for (s, v) in evs:
            if v > best.get(s, 0):
                best[s] = v
        out = []
        wd = self.waited[e]
        for s, v in best.items():
            if wd.get(s, 0) >= v:
                continue
            wd[s] = v
            out.append((s, v))
        return out

    def op(self, e, fn, reads=(), writes=(), pe_acc=False):
        evs = self._deps(reads, writes)
        ev = self._eng_event(e)
        if pe_acc:
            own = set(self.esems[e])
            wk = set(writes)
            evs2 = []
            for k in reads:
                w = self.last_w.get(k)
                if w is not None:
                    evs2.append(w)
            for k in wk:
                w = self.last_w.get(k)
                if w is not None and w[0] not in own:
                    evs2.append(w)
                evs2.extend(self.readers.get(k, ()))
            evs = evs2
        waits = self._filter(e, evs)
        self.items[e].append((waits, fn, (ev[0], 1)))
        self._commit(ev, reads, writes)
        self.last_ev[e] = ev

    def dma(self, out_ap, in_ap, reads=(), writes=(), e="sp"):
        evs = self._deps(reads, writes)
        extra = []
        ev = self._dma_event(extra)
        waits = self._filter(e, evs + extra)

        def fn(eng, out_ap=out_ap, in_ap=in_ap):
            return eng.dma_start(out=out_ap, in_=in_ap)
        self.items[e].append((waits, fn, (ev[0], 16)))
        self._commit(ev, reads, writes)

    def barrier(self):
        evs = [(s, c) for s, c in zip(self.dma_sems, self.dma_cum) if c > 0]
        evs += list(self.last_ev.values())
        for e in self.ENGS:
            waits = self._filter(e, evs)
            if waits:
                self.items[e].append((waits, None, None))
        self.last_w = {}
        self.readers = {}

    def replay(self):
        nc = self.nc
        sem = self.semobjs

        def run(eng, items):
            for waits, fn, inc in items:
                for (s, v) in waits:
                    eng.wait_ge(sem[s], v)
                if fn is None:
                    continue
                ins = fn(eng)
                ins.then_inc(sem[inc[0]], inc[1])

        with nc.Block() as block:
            @block.tensor
            def _(eng):
                run(eng, self.items["pe"])

            @block.scalar
            def _(eng):
                run(eng, self.items["act"])

            @block.vector
            def _(eng):
                run(eng, self.items["dve"])

            @block.gpsimd
            def _(eng):
                run(eng, self.items["pool"])

            @block.sync
            def _(eng):
                run(eng, self.items["sp"])


class Arena:
    def __init__(self, t2d):
        self.t = t2d
        self.off = 0
        self.n = t2d.shape[1]

    def reset(self):
        self.off = 0

    def get(self, shape):
        p = shape[0]
        n = int(np.prod(shape[1:]))
        assert self.off + n <= self.n, ("arena overflow", self.off, n, self.n)
        ap = self.t[0:p, self.off:self.off + n]
        self.off += n
        if len(shape) == 3:
            ap = ap.rearrange("p (a b) -> p a b", a=shape[1], b=shape[2])
        elif len(shape) == 4:
            ap = ap.rearrange("p (a b c) -> p a b c", a=shape[1], b=shape[2], c=shape[3])
        return ap


def t5_bucket_np(dist):
    n = np.maximum(dist, 0)
    nf = np.maximum(n, 1).astype(np.float32)
    large = 16 + (np.log(nf / np.float32(16)) / np.float32(math.log(128 / 16)) * np.float32(16)).astype(np.int32)
    return np.where(n < 16, n, np.minimum(large, 31)).astype(np.int64)


def t5_bucket_jax(dist):
    import jax.numpy as jnp
    dist = jnp.asarray(dist, jnp.int32)
    n = jnp.maximum(dist, 0)
    nf = jnp.maximum(n, 1).astype(jnp.float32)
    large = 16 + (jnp.log(nf / 16) / math.log(128 / 16) * 16).astype(jnp.int32)
    return np.asarray(jnp.where(n < 16, n, jnp.minimum(large, 31))).astype(np.int64)


def host_tables(S, rel_bias):
    bucket = t5_bucket_np
    kr = np.arange(128)[:, None]
    tr = np.arange(128)[None, :]
    d_diag = tr - kr
    d_prev = tr - kr + 128
    bk_diag = bucket(np.maximum(d_diag, 0))
    bk_prev = bucket(d_prev)
    rb = rel_bias.astype(np.float32)
    t = {}
    t["gD"] = np.ascontiguousarray(rb[bk_diag].transpose(0, 2, 1))
    t["gP"] = np.ascontiguousarray(rb[bk_prev].transpose(0, 2, 1))
    t["mD"] = np.where(d_diag >= 0, 0.0, NEG).astype(np.float32)
    t["mW"] = np.where(kr > tr, 0.0, NEG).astype(np.float32)
    trc = np.arange(128)[:, None]
    cr = np.arange(16)[None, :]
    d_c = trc - 16 * cr + 113
    t["gC"] = np.ascontiguousarray(rb[bucket(np.maximum(d_c, 0))][:, :, :8].transpose(0, 2, 1))
    t["mC"] = np.where(d_c >= 0, 0.0, NEG).astype(np.float32)
    t["r31"] = np.ascontiguousarray(np.broadcast_to(rb[31][None, :], (128, 12)))
    nsel = S // 64
    nqt = S // 128
    tt = np.arange(S)
    cur = tt // 64
    j = np.arange(64 if nsel >= 64 else nsel)
    NJ = 64
    lo = np.full((S, NJ), -1e30, np.float32)
    hi = np.full((S, NJ), 3e38, np.float32)
    jj = np.arange(NJ)[None, :]
    c2 = cur[:, None]
    lo[(jj == c2 - 1) & (jj < nsel)] = 1e9
    lo[(jj == c2) & (jj < nsel)] = 2e9
    lo[np.broadcast_to(jj == 0, lo.shape)] = 3e9
    hi[np.broadcast_to(jj, hi.shape) > c2] = -1e30
    t["LO"] = np.ascontiguousarray(lo.reshape(nqt, 128, NJ).transpose(1, 0, 2))
    t["HI"] = np.ascontiguousarray(hi.reshape(nqt, 128, NJ).transpose(1, 0, 2))
    ncmp = (S - 32) // 16 + 1
    cs = np.arange(ncmp) * 16
    ss = np.arange(nsel) * 64
    lo_ = np.maximum(cs[:, None], ss[None, :])
    hi_ = np.minimum(cs[:, None] + 32, ss[None, :] + 64)
    c2s = np.zeros((256, NJ), np.float32)
    c2s[:ncmp, :nsel] = np.maximum(hi_ - lo_, 0).astype(np.float32) / 32
    t["C2S"] = np.ascontiguousarray(c2s.reshape(2, 128, NJ).transpose(1, 0, 2))
    w2 = np.zeros((64, 64, 64), np.float32)
    for q in range(64):
        w2[q, q, :] = 1.0
    t["W2"] = w2.reshape(64, 4096)
    return t


def win_perm():
    cols = []
    cols += list(range(0, 512))
    kv = 512
    cols += list(range(kv + 0, kv + 128))
    cols += list(range(kv + 128, kv + 256))
    cols += list(range(kv + 256, kv + 384))
    cols += list(range(kv + 512, kv + 640))
    cols += list(range(1304, 1816))
    cols += list(range(1816, 2328))
    cols += list(range(kv + 384, kv + 512))
    cols += list(range(kv + 640, kv + 768))
    cols += list(range(1280, 1304))
    cols += list(range(2328, 2840))
    assert len(cols) == 2840 and len(set(cols)) == 2840
    return np.array(cols)


def build(NB, S, debug=(), stages=None, TP=256):
    nc = bass.Bass("TRN2", target_bir_lowering=False)
    T = NB * S
    NQT = S // 128
    NBLK = S // 512
    C = S // 16 - 1
    NCT = (C + 127) // 128
    all_stages = ["pre", "mod", "proj", "cmp", "csel", "sel", "win", "diff", "comb", "peer"]
    stages = all_stages if stages is None else stages

    def din(name, shape, dt=F32):
        return nc.dram_tensor(name, list(shape), dt, kind="ExternalInput").ap()

    def dscr(name, shape, dt=F32):
        kind = "ExternalOutput" if name in debug else "Internal"
        return nc.dram_tensor(name, list(shape), dt, kind=kind).ap()

    x_d = din("x", [T, D])
    cT_d = din("cT", [128, 8, NB])
    adaw_d = din("ada_w", [8, 128, 6144])
    adabT_d = din("ada_bT", [128, 48])
    adab_d = din("ada_b", [1, 6144])
    g12T_d = din("g12T", [128, 2, 8])
    win_d = din("w_in", [8, 128, 2840])
    wout_d = din("w_out", [8, 128, 1024])
    posT_d = din("posT", [64, 2, 32])
    w1_d = din("w1r", [2, 64, 32, 256])
    w2_d = din("w2r", [128, 2, 2, 64])
    lam_d = din("lamv", [4, 64])
    subg_d = din("subg", [1, 128])
    wq_d = din("wq", [8, 128, 2048])
    skT_d = din("skT", [128, 2, 128])
    uT_d = din("uT", [128, 128, 8, 128])
    v_d = din("v", [128, 128, 1024])
    fing_d = din("fing", [1, 1024])
    gD_d = din("gD", [128, 12, 128]); gP_d = din("gP", [128, 12, 128])
    mD_d = din("mD", [128, 128]); mW_d = din("mW", [128, 128])
    gC_d = din("gC", [128, 8, 16]); mC_d = din("mC", [128, 16]); r31_d = din("r31", [128, 12])
    LO_d = din("LO", [128, NQT, 64]); HI_d = din("HI", [128, NQT, 64])
    C2S_d = din("C2S", [128, 2, 64]); W2_d = din("W2", [64, 4096])
    out_d = nc.dram_tensor("out", [T, D], F32, kind="ExternalOutput").ap()

    FT = dscr("FT", [16, 128, T], BF)
    TMV = dscr("TMV", [T, 256], BF)
    GT = dscr("GT", [T, 24])
    DV = dscr("DV", [T, 512], BF)
    KC = dscr("KC", [NB, 2, 64, 256], BF)
    VC = dscr("VC", [NB, 2, 256, 64])
    OC = dscr("OC", [T, 512]); OS = dscr("OS", [T, 512]); OW = dscr("OW", [T, 512]); OD = dscr("OD", [T, 512])
    NM = dscr("NM", [NB, 2, 64, S], BF)
    X1 = dscr("X1", [T, D])
    UB = dscr("UB", [128, 128, 8, 128], BF)
    VB = dscr("VB", [128, 128, 1024], BF)
    MODD = dscr("MODD", [128, 64, NB])

    with ExitStack() as st:
        P = Planner(nc, st)
        NF = 22 * 1024
        NBF = 52 * 1024
        NU = NF + NBF // 2
        arU_t = st.enter_context(nc.sbuf_tensor("arU", [128, NU], F32))
        pers_t = st.enter_context(nc.sbuf_tensor("pers", [128, 512], F32))
        persb_t = st.enter_context(nc.sbuf_tensor("persb", [128, 256], BF))
        psum_t = st.enter_context(nc.psum_tensor("psum", [128, 4096], F32))
        aF = Arena(arU_t[:, 0:NF]); aB = Arena(arU_t[:, NF:NU].bitcast(BF)); aP = Arena(pers_t[:]); aPb = Arena(persb_t[:])

        def set_split(nf):
            aF.t = arU_t[:, 0:nf]; aF.n = nf; aF.off = 0
            aB.t = arU_t[:, nf:NU].bitcast(BF); aB.n = (NU - nf) * 2; aB.off = 0

        def bank(i, n=512):
            return psum_t[:, i * 512:i * 512 + n]

        def _stage(name):
            def deco(f):
                if name in stages:
                    f()
                return f
            return deco

        ident = aP.get([128, 128]); epsb = aP.get([128, 1])
        A1T = aP.get([128, 8, NB]); B1T = aP.get([128, 8, NB]); A2T = aP.get([128, 8, NB]); B2T = aP.get([128, 8, NB])
        neglam = aP.get([128, 1])
        identb = aPb.get([128, 128])
        MODB = dscr("MODB", [NB, 2, 128, 1024])

        P.op("pool", lambda e: e.memset(ident, 1.0), writes=["ident"])
        P.op("pool", lambda e: e.affine_select(out=ident, in_=ident, pattern=[[-1, 128]], compare_op=ALU.is_equal,
                                               fill=0.0, base=0, channel_multiplier=1), reads=["ident"], writes=["ident"])
        P.op("pool", lambda e: e.memset(epsb, 1e-6), writes=["epsb"])
        P.op("dve", lambda e: e.tensor_copy(out=identb, in_=ident), reads=["ident"], writes=["identb"])

        evac_rr = [0]

        def evac(out, in_, reads, writes, scale=None):
            evac_rr[0] ^= 1
            if evac_rr[0]:
                if scale is None:
                    P.op("act", lambda e: e.activation(out=out, in_=in_, func=AF.Copy), reads=reads, writes=writes)
                else:
                    P.op("act", lambda e: e.activation(out=out, in_=in_, func=AF.Copy, scale=scale), reads=reads, writes=writes)
            else:
                if scale is None:
                    P.op("dve", lambda e: e.tensor_copy(out=out, in_=in_), reads=reads, writes=writes)
                else:
                    P.op("dve", lambda e: e.tensor_scalar(out=out, in0=in_, scalar1=scale, scalar2=None, op0=ALU.mult),
                         reads=reads, writes=writes)

        def load_cast(dst_bf, src_dram, stage_f32, key_dst, key_stage, eng="pool"):
            P.dma(stage_f32, src_dram, writes=[key_stage])
            P.op(eng, lambda e: e.tensor_copy(out=dst_bf, in_=stage_f32), reads=[key_stage], writes=[key_dst])

        def rms_rows(xt, nsub, ssq, rstd, scr, kx, tag):
            for s in range(nsub):
                P.op("act", lambda e, s=s: e.activation(out=scr, in_=xt[:, s, :], func=AF.Square, accum_out=ssq[:, s:s + 1]),
                     reads=[kx], writes=[tag + "scr", tag + "ssq"])
            P.op("act", lambda e: e.activation(out=rstd, in_=ssq, func=AF.Sqrt, scale=1.0 / D, bias=epsb),
                 reads=[tag + "ssq", "epsb"], writes=[tag + "rstd"])
            P.op("dve", lambda e: e.reciprocal(out=rstd, in_=rstd), reads=[tag + "rstd"], writes=[tag + "rstd"])

        @_stage("pre")
        def _st():
            aF.reset(); aB.reset()
            G = 4
            stg = [aF.get([128, G, 1024]) for _ in range(2)]
            stb = [aB.get([128, G, 1024]) for _ in range(2)]
            it = 0
            for (src, dst, is_u) in ((uT_d, UB, True), (v_d, VB, False)):
                for ig in range(128 // G):
                    r = it % 2
                    it += 1
                    if is_u:
                        s_ap = src[ig * G:(ig + 1) * G].rearrange("i p k e -> p i (k e)")
                        d_ap = dst[ig * G:(ig + 1) * G].rearrange("i p k e -> p i (k e)")
                    else:
                        s_ap = src[ig * G:(ig + 1) * G].rearrange("i p n -> p i n")
                        d_ap = dst[ig * G:(ig + 1) * G].rearrange("i p n -> p i n")
                    P.dma(stg[r], s_ap, writes=[f"stg{r}"])
                    eng = ("pool", "dve", "act")[it % 3]
                    if eng == "act":
                        P.op("act", lambda e, r=r: e.activation(out=stb[r], in_=stg[r], func=AF.Copy), reads=[f"stg{r}"], writes=[f"stb{r}"])
                    else:
                        P.op(eng, lambda e, r=r: e.tensor_copy(out=stb[r], in_=stg[r]), reads=[f"stg{r}"], writes=[f"stb{r}"])
                    P.dma(d_ap, stb[r], reads=[f"stb{r}"], writes=["UBVB"])
            P.barrier()

        @_stage("mod")
        def _st():
            aF.reset(); aB.reset()
            condT = aF.get([128, 8, NB])
            crep = aF.get([128, 8 * NB, 128])
            adab_T = aF.get([128, 48])
            g12T = aF.get([128, 2, 8])
            modT = aF.get([128, 48, NB])
            adab_bc = aF.get([128, 2, 1024])
            wall = aF.get([128, 8, 2048])
            gout = aF.get([128, 1024])
            P.dma(condT, cT_d, writes=["condT"])
            P.dma(adab_T, adabT_d, writes=["adabT"])
            P.dma(g12T, g12T_d, writes=["g12T"])
            P.dma(adab_bc[:, 0, :], adab_d[:, 2048:3072].partition_broadcast(128), writes=["adab_bc"])
            P.dma(adab_bc[:, 1, :], adab_d[:, 5120:6144].partition_broadcast(128), writes=["adab_bc"])
            P.op("act", lambda e: e.activation(out=condT, in_=condT, func=AF.Silu), reads=["condT"], writes=["condT"])
            P.op("dve", lambda e: e.tensor_copy(out=crep, in_=condT.rearrange("p k b -> p (k b)").unsqueeze(2).to_broadcast([128, 8 * NB, 128])),
                 reads=["condT"], writes=["crep"])
            pm = bank(0)
            mlist = list(range(0, 16)) + list(range(24, 40))
            for half, cols0 in ((0, 0), (1, 3072)):
                for k in range(8):
                    P.dma(wall[:, k, :], adaw_d[k, :, cols0:cols0 + 2048], writes=["wall"])
                for mi in range(16):
                    m = (0 if half == 0 else 24) + mi
                    for k in range(8):
                        P.op("pe", lambda e, mi=mi, m=m, k=k: e.matmul(out=pm[:, m * NB:(m + 1) * NB], lhsT=wall[:, k, mi * 128:(mi + 1) * 128],
                                                                 rhs=condT[:, k, :], start=(k == 0), stop=(k == 7)),
                             reads=["wall", "condT"], writes=["bank0"], pe_acc=(k > 0))
            P.op("dve", lambda e: e.tensor_tensor(out=modT, in0=pm[:, 0:48 * NB].rearrange("p (m b) -> p m b", b=NB),
                                                  in1=adab_T.unsqueeze(2).to_broadcast([128, 48, NB]), op=ALU.add),
                 reads=["bank0", "adabT"], writes=["modT"])
            for (At, Bt, gi, msh, msc) in ((A1T, B1T, 0, 0, 8), (A2T, B2T, 1, 24, 32)):
                P.op("dve", lambda e, At=At, msc=msc: e.tensor_scalar(out=At, in0=modT[:, msc:msc + 8, :], scalar1=1.0, scalar2=None, op0=ALU.add),
                     reads=["modT"], writes=["AB"])
                P.op("dve", lambda e, At=At, gi=gi: e.tensor_tensor(out=At, in0=At, in1=g12T[:, gi, :].unsqueeze(2).to_broadcast([128, 8, NB]), op=ALU.mult),
                     reads=["AB", "g12T"], writes=["AB"])
                P.op("dve", lambda e, Bt=Bt, msh=msh: e.tensor_copy(out=Bt, in_=modT[:, msh:msh + 8, :]), reads=["modT"], writes=["AB"])
            for b in range(NB):
                for gi, c0 in ((0, 2048), (1, 5120)):
                    for k in range(8):
                        P.dma(wall[:, k, 0:1024], adaw_d[k, :, c0:c0 + 1024], writes=["wall"])
                    for nb2 in range(2):
                        for k in range(8):
                            P.op("pe", lambda e, nb2=nb2, k=k, b=b: e.matmul(out=bank(1 + nb2), lhsT=crep[:, k * NB + b, :],
                                                                      rhs=wall[:, k, nb2 * 512:(nb2 + 1) * 512], start=(k == 0), stop=(k == 7)),
                                 reads=["wall", "crep"], writes=[f"bank{1 + nb2}"], pe_acc=(k > 0))
                    for nb2 in range(2):
                        P.op("dve", lambda e, nb2=nb2, gi=gi: e.tensor_tensor(out=gout[:, nb2 * 512:(nb2 + 1) * 512], in0=bank(1 + nb2),
                                                                       in1=adab_bc[:, gi, nb2 * 512:(nb2 + 1) * 512], op=ALU.add),
                             reads=[f"bank{1 + nb2}", "adab_bc"], writes=["gout"])
                    P.dma(MODB[b, gi], gout, reads=["gout"], writes=["MODB"])
            if "MODD" in debug:
                P.dma(MODD[:, 0:48, :], modT, reads=["modT"], writes=["MODD"])
            lamt = aF.get([128, 4, 64]); lprod = aF.get([128, 2, 64]); lsum = aF.get([128, 2])
            P.dma(lamt.rearrange("p a b -> p (a b)"), lam_d.rearrange("a b -> (a b)").unsqueeze(0).partition_broadcast(128) if False else
                  lam_d.rearrange("(o a) b -> o (a b)", o=1).partition_broadcast(128), writes=["lamt"])
            P.op("dve", lambda e: e.tensor_tensor(out=lprod[:, 0, :], in0=lamt[:, 0, :], in1=lamt[:, 1, :], op=ALU.mult), reads=["lamt"], writes=["lprod"])
            P.op("dve", lambda e: e.tensor_tensor(out=lprod[:, 1, :], in0=lamt[:, 2, :], in1=lamt[:, 3, :], op=ALU.mult), reads=["lprod", "lamt"], writes=["lprod"])
            P.op("dve", lambda e: e.tensor_reduce(out=lsum, in_=lprod, axis=AX.X, op=ALU.add), reads=["lprod"], writes=["lsum"])
            P.op("act", lambda e: e.activation(out=lsum, in_=lsum, func=AF.Exp), reads=["lsum"], writes=["lsum"])
            P.op("dve", lambda e: e.tensor_tensor(out=neglam, in0=lsum[:, 1:2], in1=lsum[:, 0:1], op=ALU.subtract), reads=["lsum"], writes=["neglam"])
            P.op("dve", lambda e: e.tensor_scalar(out=neglam, in0=neglam, scalar1=-0.2, scalar2=None, op0=ALU.add), reads=["neglam"], writes=["neglam"])
            P.barrier()

        @_stage("proj")
        def _st():
            aF.reset(); aB.reset()
            win = aB.get([128, 8, 2840])
            wst = [aF.get([128, 2840]) for _ in range(2)]
            for k in range(8):
                load_cast(win[:, k, :], win_d[k], wst[k % 2], "win", f"wst{k % 2}", eng=("pool", "dve")[k % 2])
            xt = aF.get([128, 4, 1024]); xn = aF.get([128, 4, 1024]); scr = aF.get([128, 1024])
            ssq = aF.get([128, 4]); rstd = aF.get([128, 4])
            hT = aB.get([128, 8, 512])
            ftsb = aB.get([128, 16, 512])
            tmv = aB.get([128, 4, 256]); dvs = aB.get([128, 4, 512])
            gts = aF.get([128, 4, 24])
            for b in range(NB):
                for blk in range(NBLK):
                    r0 = b * S + blk * 512
                    P.dma(xt, x_d[r0:r0 + 512, :].rearrange("(s p) d -> p s d", p=128), writes=["xt"])
                    rms_rows(xt, 4, ssq, rstd, scr, "xt", "p1")
                    for s in range(4):
                        P.op("dve", lambda e, s=s: e.tensor_scalar(out=xn[:, s, :], in0=xt[:, s, :], scalar1=rstd[:, s:s + 1], scalar2=None, op0=ALU.mult),
                             reads=["xt", "p1rstd"], writes=[f"xn{s}"])
                    for k in range(8):
                        bk = k % 2
                        for s in range(4):
                            P.op("pe", lambda e, s=s, k=k, bk=bk: e.transpose(out=bank(bk)[:, s * 128:(s + 1) * 128], in_=xn[:, s, k * 128:(k + 1) * 128], identity=ident),
                                 reads=[f"xn{s}", "ident"], writes=[f"bank{bk}"], pe_acc=(s > 0))
                        P.op("act", lambda e, k=k, bk=bk, b=b: e.activation(out=hT[:, k, :], in_=bank(bk), func=AF.Identity, scale=A1T[:, k, b:b + 1], bias=B1T[:, k, b:b + 1]),
                             reads=[f"bank{bk}"], writes=[f"hT{k}"])
                    hkeys = [f"hT{k}" for k in range(8)]
                    for c in range(16):
                        bk = 2 + c % 3
                        for k in range(8):
                            P.op("pe", lambda e, c=c, k=k, bk=bk: e.matmul(out=bank(bk), lhsT=win[:, k, c * 128:(c + 1) * 128], rhs=hT[:, k, :], start=(k == 0), stop=(k == 7)),
                                 reads=["win"] + hkeys, writes=[f"bank{bk}"], pe_acc=(k > 0))
                        sc_ = 0.125 if (c < 4 or 8 <= c < 12) else None
                        evac(ftsb[:, c, :], bank(bk), [f"bank{bk}"], ["ftsb"], scale=sc_)
                    P.dma(FT[:, :, r0:r0 + 512].rearrange("c p t -> p c t"), ftsb, reads=["ftsb"], writes=["FT"])
                    for s in range(4):
                        ba, bb = 5 + (s % 2), 7
                        for k in range(8):
                            P.op("pe", lambda e, s=s, k=k, ba=ba: e.matmul(out=bank(ba)[:, 0:280], lhsT=hT[:, k, s * 128:(s + 1) * 128], rhs=win[:, k, 2048:2328], start=(k == 0), stop=(k == 7)),
                                 reads=["win"] + hkeys, writes=[f"bank{ba}"], pe_acc=(k > 0))
                        for k in range(8):
                            P.op("pe", lambda e, s=s, k=k: e.matmul(out=bank(7), lhsT=hT[:, k, s * 128:(s + 1) * 128], rhs=win[:, k, 2328:2840], start=(k == 0), stop=(k == 7)),
                                 reads=["win"] + hkeys, writes=["bank7"], pe_acc=(k > 0))
                        P.op("dve", lambda e, s=s, ba=ba: e.tensor_copy(out=tmv[:, s, :], in_=bank(ba)[:, 0:256]), reads=[f"bank{ba}"], writes=["tmv"])
                        P.op("act", lambda e, s=s, ba=ba: e.activation(out=gts[:, s, :], in_=bank(ba)[:, 256:280], func=AF.Sigmoid), reads=[f"bank{ba}"], writes=["gts"])
                        evac(dvs[:, s, :], bank(7), ["bank7"], ["dvs"])
                    P.dma(TMV[r0:r0 + 512, :].rearrange("(s p) c -> p s c", p=128), tmv, reads=["tmv"], writes=["TMV"])
                    P.dma(GT[r0:r0 + 512, :].rearrange("(s p) c -> p s c", p=128), gts, reads=["gts"], writes=["GT"])
                    P.dma(DV[r0:r0 + 512, :].rearrange("(s p) c -> p s c", p=128), dvs, reads=["dvs"], writes=["DV"])
            P.barrier()

        @_stage("cmp")
        def _st():
            aF.reset(); aB.reset()
            w1b = aB.get([64, 2, 32 * 256])
            w1st = aF.get([64, 32 * 256])
            for kv in range(2):
                load_cast(w1b[:, kv, :], w1_d[kv].rearrange("d l h -> d (l h)"), w1st, "w1b", "w1st")
            posf = aF.get([64, 64]); posb = aB.get([64, 64])
            load_cast(posb, posT_d.rearrange("d a l -> d (a l)"), posf, "posb", "posf", eng="dve")
            w2f = aF.get([128, 256]); w2b = aB.get([128, 2, 2, 64])
            load_cast(w2b.rearrange("p a b c -> p (a b c)"), w2_d.rearrange("p a b c -> p (a b c)"), w2f, "w2b", "w2f", eng="dve")
            hb = aF.get([128, 4])
            for kv in range(2):
                for half in range(2):
                    for l in range(32):
                        P.op("pe", lambda e, kv=kv, half=half, l=l: e.matmul(out=bank(0)[:, kv * 2 + half:kv * 2 + half + 1],
                                                                      lhsT=w1b[:, kv, l * 256 + half * 128:l * 256 + half * 128 + 128],
                                                                      rhs=posb[:, kv * 32 + l:kv * 32 + l + 1], start=(l == 0), stop=(l == 31)),
                             reads=["w1b", "posb"], writes=["bank0"], pe_acc=not (kv == 0 and half == 0 and l == 0))
            P.op("dve", lambda e: e.tensor_copy(out=hb, in_=bank(0)[:, 0:4]), reads=["bank0"], writes=["hb"])
            tok = aB.get([64, S]); hid = aB.get([128, 2, 256])
            kcs = aB.get([64, 256]); vcs = aF.get([128, 2, 64])
            for b in range(NB):
                for g in range(2):
                    for kv in range(2):
                        P.dma(tok, FT[4 + kv, g * 64:(g + 1) * 64, b * S:(b + 1) * S], reads=["FT"], writes=["tok"])
                        for half in range(2):
                            bk = 1 + half
                            for l in range(32):
                                P.op("pe", lambda e, kv=kv, half=half, l=l, bk=bk: e.matmul(out=bank(bk)[:, 0:C], lhsT=w1b[:, kv, l * 256 + half * 128:l * 256 + half * 128 + 128],
                                                                                     rhs=tok[:, l:l + 16 * (C - 1) + 1:16], start=(l == 0), stop=(l == 31)),
                                     reads=["w1b", "tok"], writes=[f"bank{bk}"], pe_acc=(l > 0))
                            P.op("act", lambda e, kv=kv, half=half, bk=bk: e.activation(out=hid[:, half, 0:C], in_=bank(bk)[:, 0:C], func=AF.Gelu_apprx_tanh,
                                                                                   bias=hb[:, kv * 2 + half:kv * 2 + half + 1]),
                                 reads=[f"bank{bk}", "hb"], writes=[f"hid{half}"])
                        if kv == 0:
                            for half in range(2):
                                P.op("pe", lambda e, half=half: e.matmul(out=bank(3)[0:64, 0:C], lhsT=w2b[:, 0, half, :], rhs=hid[:, half, 0:C], start=(half == 0), stop=(half == 1)),
                                     reads=["w2b", f"hid{half}"], writes=["bank3"], pe_acc=(half > 0))
                            P.op("pool", lambda e: e.memset(kcs, 0.0), writes=["kcs"])
                            P.op("dve", lambda e: e.tensor_copy(out=kcs[:, 0:C], in_=bank(3)[0:64, 0:C]), reads=["bank3"], writes=["kcs"])
                            P.dma(KC[b, g], kcs, reads=["kcs"], writes=["KC"])
                        else:
                            P.op("pool", lambda e: e.memset(vcs, 0.0), writes=["vcs"])
                            for ct in range(NCT):
                                cs = min(128, C - ct * 128)
                                for half in range(2):
                                    P.op("pe", lambda e, ct=ct, cs=cs, half=half: e.matmul(out=bank(4)[0:cs, ct * 64:(ct + 1) * 64], lhsT=hid[:, half, ct * 128:ct * 128 + cs],
                                                                                    rhs=w2b[:, 1, half, :], start=(half == 0), stop=(half == 1)),
                                         reads=["w2b", f"hid{half}"], writes=["bank4"], pe_acc=(half > 0 or ct > 0))
                            for ct in range(NCT):
                                cs = min(128, C - ct * 128)
                                P.op("dve", lambda e, ct=ct, cs=cs: e.tensor_copy(out=vcs[0:cs, ct, :], in_=bank(4)[0:cs, ct * 64:(ct + 1) * 64]), reads=["bank4"], writes=["vcs"])
                            P.dma(VC[b, g].rearrange("(ct p) d -> p ct d", p=128), vcs, reads=["vcs"], writes=["VC"])
            P.barrier()

        need_attn = any(s in stages for s in ("csel", "sel", "win", "diff"))
        if need_attn:
            aF.reset(); aB.reset()
            bD = aB.get([128, 12, 128]); bP = aB.get([128, 12, 128]); bW = aB.get([128, 4, 128])
            tf = aF.get([128, 12, 128]); r31 = aF.get([128, 12]); mDt = aF.get([128, 128])
            P.dma(r31, r31_d, writes=["r31"])
            P.dma(mDt, mD_d, writes=["mDt"])
            P.dma(tf, gD_d, writes=["tf"])
            P.op("dve", lambda e: e.tensor_tensor(out=tf, in0=tf, in1=r31.unsqueeze(2).to_broadcast([128, 12, 128]), op=ALU.subtract), reads=["tf", "r31"], writes=["tf"])
            P.op("dve", lambda e: e.tensor_tensor(out=bD, in0=tf, in1=mDt.unsqueeze(1).to_broadcast([128, 12, 128]), op=ALU.add), reads=["tf", "mDt"], writes=["bD"])
            P.dma(tf, gP_d, reads=["tf"], writes=["tf"])
            P.op("dve", lambda e: e.tensor_tensor(out=bP, in0=tf, in1=r31.unsqueeze(2).to_broadcast([128, 12, 128]), op=ALU.subtract), reads=["tf", "r31"], writes=["bP"])
            mWt = aF.get([128, 128])
            P.dma(mWt, mW_d, writes=["mWt"])
            P.op("dve", lambda e: e.tensor_copy(out=bW, in_=mWt.unsqueeze(1).to_broadcast([128, 4, 128])), reads=["mWt"], writes=["bW"])
            bC = aF.get([128, 8, 16]); mCt = aF.get([128, 16])
            P.dma(bC, gC_d, writes=["bC"])
            P.dma(mCt, mC_d, writes=["mCt"])
            P.op("dve", lambda e: e.tensor_tensor(out=bC, in0=bC, in1=r31[:, 0:8].unsqueeze(2).to_broadcast([128, 8, 16]), op=ALU.subtract), reads=["bC", "r31"], writes=["bC"])
            P.op("dve", lambda e: e.tensor_tensor(out=bC, in0=bC, in1=mCt.unsqueeze(1).to_broadcast([128, 8, 16]), op=ALU.add), reads=["bC", "mCt"], writes=["bC"])
            attnF0, attnB0 = aF.off, aB.off

        @_stage("csel")
        def _st():
            aF.off, aB.off = attnF0, attnB0
            LO = aF.get([128, NQT, 64]); HI = aF.get([128, NQT, 64]); C2S = aF.get([128, 2, 64])
            P.dma(LO, LO_d, writes=["LO"]); P.dma(HI, HI_d, writes=["HI"]); P.dma(C2S, C2S_d, writes=["C2S"])
            kc = aB.get([64, 256]); vc = aF.get([128, 2, 64])
            qh = aB.get([64, 4, 128])
            pc = aF.get([128, 4, 256]); pT = aF.get([128, 2, 512])
            mx = aF.get([128, 4]); Z = aF.get([128, 4])
            scs = aF.get([128, 64]); rep = aF.get([128, 64]); m8 = aF.get([128, 16]); nm = aF.get([128, 64])
            ocs = aF.get([128, 256]); nmT = aB.get([64, S])
            pS = psum_t[:, 0:1024].rearrange("p (h c) -> p h c", c=256)
            for b in range(NB):
                for g in range(2):
                    P.dma(kc, KC[b, g], reads=["KC"], writes=["kc"])
                    P.dma(vc, VC[b, g].rearrange("(ct p) d -> p ct d", p=128), reads=["VC"], writes=["vc"])
                    for qt in range(NQT):
                        t0 = b * S + qt * 128
                        ncols = min(8 * qt + 7, C)
                        nct = (ncols + 127) // 128
                        P.dma(qh, FT[2 * g:2 * g + 2, :, t0:t0 + 128].rearrange("c (two d) t -> d (c two) t", two=2), reads=["FT"], writes=["qh"])
                        for hh in range(4):
                            P.op("pe", lambda e, hh=hh, ncols=ncols: e.matmul(out=pS[:, hh, 0:ncols], lhsT=qh[:, hh, :], rhs=kc[:, 0:ncols], start=True, stop=True),
                                 reads=["qh", "kc"], writes=[f"bank{hh // 2}"])
                        cw0 = 8 * qt - 9
                        w0 = max(cw0, 0)
                        P.op("dve", lambda e, w0=w0, cw0=cw0, ncols=ncols, g=g: e.tensor_tensor(out=pS[:, :, w0:ncols], in0=pS[:, :, w0:ncols],
                                                                                         in1=bC[:, 4 * g:4 * g + 4, w0 - cw0:ncols - cw0], op=ALU.add),
                             reads=["bank0", "bank1", "bC"], writes=["bank0", "bank1"])
                        P.op("dve", lambda e, ncols=ncols: e.tensor_reduce(out=mx, in_=pS[:, :, 0:ncols], axis=AX.X, op=ALU.max), reads=["bank0", "bank1"], writes=["mx"])
                        P.op("dve", lambda e: e.tensor_scalar(out=mx, in0=mx, scalar1=-1000.0, scalar2=-1.0, op0=ALU.max, op1=ALU.mult), reads=["mx"], writes=["mx"])
                        for hh in range(4):
                            P.op("act", lambda e, hh=hh, ncols=ncols: e.activation(out=pc[:, hh, 0:ncols], in_=pS[:, hh, 0:ncols], func=AF.Exp, bias=mx[:, hh:hh + 1], accum_out=Z[:, hh:hh + 1]),
                                 reads=[f"bank{hh // 2}", "mx"], writes=["pc", "Z"])
                        P.op("dve", lambda e: e.tensor_scalar(out=Z, in0=Z, scalar1=1e-30, scalar2=None, op0=ALU.add), reads=["Z"], writes=["Z"])
                        P.op("dve", lambda e: e.reciprocal(out=Z, in_=Z), reads=["Z"], writes=["Z"])
                        P.op("dve", lambda e, ncols=ncols: e.tensor_tensor(out=pc[:, :, 0:ncols], in0=pc[:, :, 0:ncols], in1=Z.unsqueeze(2).to_broadcast([128, 4, ncols]), op=ALU.mult),
                             reads=["pc", "Z"], writes=["pc"])
                        for ct in range(nct):
                            cs = min(128, ncols - ct * 128)
                            for hh in range(4):
                                P.op("pe", lambda e, ct=ct, cs=cs, hh=hh: e.transpose(out=bank(2 + ct)[0:cs, hh * 128:(hh + 1) * 128], in_=pc[:, hh, ct * 128:ct * 128 + cs], identity=ident),
                                     reads=["pc", "ident"], writes=[f"bank{2 + ct}"], pe_acc=(hh > 0))
                            evac(pT[0:cs, ct, :], bank(2 + ct)[0:cs, :], [f"bank{2 + ct}"], ["pT"])
                        first = True
                        for ct in range(nct):
                            cs = min(128, ncols - ct * 128)
                            for hh in range(4):
                                last = (ct == nct - 1 and hh == 3)
                                P.op("pe", lambda e, ct=ct, cs=cs, hh=hh, first=first, last=last: e.matmul(out=bank(4)[:, 0:64], lhsT=pT[0:cs, ct, hh * 128:(hh + 1) * 128], rhs=C2S[0:cs, ct, :], start=first, stop=last),
                                     reads=["pT", "C2S"], writes=["bank4"], pe_acc=not first)
                                first = False
                        for hh in range(4):
                            for ct in range(nct):
                                cs = min(128, ncols - ct * 128)
                                P.op("pe", lambda e, ct=ct, cs=cs, hh=hh, nct=nct: e.matmul(out=bank(5)[:, hh * 64:(hh + 1) * 64], lhsT=pT[0:cs, ct, hh * 128:(hh + 1) * 128], rhs=vc[0:cs, ct, :], start=(ct == 0), stop=(ct == nct - 1)),
                                     reads=["pT", "vc"], writes=["bank5"], pe_acc=not (hh == 0 and ct == 0))
                        P.op("act", lambda e: e.activation(out=ocs, in_=bank(5)[:, 0:256], func=AF.Copy), reads=["bank5"], writes=["ocs"])
                        P.dma(OC[t0:t0 + 128, g * 256:(g + 1) * 256], ocs, reads=["ocs"], writes=["OC"])
                        P.op("dve", lambda e, qt=qt: e.tensor_tensor(out=scs, in0=bank(4)[:, 0:64], in1=LO[:, qt, :], op=ALU.max), reads=["bank4", "LO"], writes=["scs"])
                        P.op("dve", lambda e, qt=qt: e.tensor_tensor(out=scs, in0=scs, in1=HI[:, qt, :], op=ALU.min), reads=["scs", "HI"], writes=["scs"])
                        P.op("dve", lambda e: e.max(out=m8[:, 0:8], in_=scs), reads=["scs"], writes=["m8"])
                        P.op("dve", lambda e: e.match_replace(out=rep, in_to_replace=m8[:, 0:8], in_values=scs, imm_value=-3e38), reads=["scs", "m8"], writes=["rep"])
                        P.op("dve", lambda e: e.max(out=m8[:, 8:16], in_=rep), reads=["rep", "m8"], writes=["m8"])
                        P.op("dve", lambda e: e.tensor_scalar(out=nm, in0=scs, scalar1=m8[:, 15:16], scalar2=NEG, op0=ALU.is_lt, op1=ALU.mult), reads=["scs", "m8"], writes=["nm"])
                        P.op("pe", lambda e: e.transpose(out=bank(6)[0:64, 0:128], in_=nm, identity=ident), reads=["nm", "ident"], writes=["bank6"])
                        P.op("act", lambda e, qt=qt: e.activation(out=nmT[:, qt * 128:(qt + 1) * 128], in_=bank(6)[0:64, 0:128], func=AF.Copy), reads=["bank6"], writes=["nmT"])
                    P.dma(NM[b, g], nmT, reads=["nmT"], writes=["NM"])
            P.barrier()

        def nsa_branch(branch):
            aF.off, aB.off = attnF0, attnB0
            is_sel = (branch == 1)
            kT = aB.get([64, S]); vs = aB.get([128, NQT, 65]); qhs = [aB.get([64, 512]) for _ in range(2)]
            pTs = [aB.get([128, 512]) for _ in range(3)]
            nmS = aB.get([64, S]) if is_sel else None
            nmrep = aB.get([64, 4, 128]) if is_sel else None
            W2 = None
            if is_sel:
                W2 = aB.get([64, 4096]); w2f = aF.get([64, 4096])
                load_cast(W2, W2_d, w2f, "W2", "w2f", eng="dve")
            rzs = [aF.get([128, 4]) for _ in range(2)]; osbs = [aF.get([128, 4, 64]) for _ in range(2)]
            ODST = OS if is_sel else OW
            P.op("pool", lambda e: e.memset(vs[:, :, 64:65], 1.0), writes=["vs1"])
            for b in range(NB):
                for g in range(2):
                    P.dma(kT, FT[5 + branch, g * 64:(g + 1) * 64, b * S:(b + 1) * S], reads=["FT"], writes=["kT"])
                    P.dma(vs[:, :, 0:64], TMV[b * S:(b + 1) * S, (branch - 1) * 128 + g * 64:(branch - 1) * 128 + (g + 1) * 64].rearrange("(kt p) d -> p kt d", p=128),
                          reads=["TMV"], writes=["vs"])
                    if is_sel:
                        P.dma(nmS, NM[b, g], reads=["NM"], writes=["nmS"])
                    for qt in range(NQT):
                        t0 = b * S + qt * 128
                        qp = qt % 2
                        qh = qhs[qp]; rz = rzs[qp]; osb = osbs[qp]; qk = f"qh{qp}"
                        P.dma(qh.rearrange("d (h t) -> d h t", h=4), FT[2 * g:2 * g + 2, :, t0:t0 + 128].rearrange("c (two d) t -> d (c two) t", two=2), reads=["FT"], writes=[qk])
                        if is_sel:
                            P.op("pool", lambda e, qt=qt: e.tensor_copy(out=nmrep, in_=nmS[:, qt * 128:(qt + 1) * 128].unsqueeze(1).to_broadcast([64, 4, 128])),
                                 reads=["nmS"], writes=["nmrep"])
                        kts = list(range(0, qt + 1)) if is_sel else list(range(max(0, qt - 4), qt + 1))
                        for ii, kt in enumerate(kts):
                            bk = ii % 3
                            pk = f"pT{bk}"
                            mm = [(kT[:, kt * 128:(kt + 1) * 128], qh, ["kT", qk])]
                            if kt == qt:
                                mm.append((identb, bD[:, 4 * g:4 * g + 4, :].rearrange("p h t -> p (h t)"), ["identb", "bD"]))
                            elif kt == qt - 1:
                                mm.append((identb, bP[:, 4 * g:4 * g + 4, :].rearrange("p h t -> p (h t)"), ["identb", "bP"]))
                            elif (not is_sel) and kt == qt - 4:
                                mm.append((identb, bW.rearrange("p h t -> p (h t)"), ["identb", "bW"]))
                            if is_sel:
                                mm.append((W2[:, kt * 128:(kt + 1) * 128], nmrep.rearrange("p h t -> p (h t)"), ["W2", "nmrep"]))
                            for mi, (l_, r_, rd) in enumerate(mm):
                                P.op("pe", lambda e, l_=l_, r_=r_, mi=mi, n=len(mm), bk=bk: e.matmul(out=bank(bk), lhsT=l_, rhs=r_, start=(mi == 0), stop=(mi == n - 1)),
                                     reads=rd, writes=[f"bank{bk}"], pe_acc=(mi > 0))
                            P.op("act", lambda e, bk=bk: e.activation(out=pTs[bk], in_=bank(bk), func=AF.Exp), reads=[f"bank{bk}"], writes=[pk])
                            for hh in range(4):
                                P.op("pe", lambda e, hh=hh, bk=bk, kt=kt, ii=ii, n=len(kts): e.matmul(out=bank(3 + hh)[:, 0:65], lhsT=pTs[bk][:, hh * 128:(hh + 1) * 128], rhs=vs[:, kt, :],
                                                                                                start=(ii == 0), stop=(ii == n - 1)),
                                     reads=[pk, "vs", "vs1"], writes=[f"bank{3 + hh}"], pe_acc=(ii > 0))
                        for hh in range(4):
                            P.op("dve", lambda e, hh=hh, rz=rz: e.reciprocal(out=rz[:, hh:hh + 1], in_=bank(3 + hh)[:, 64:65]), reads=[f"bank{3 + hh}"], writes=[f"rz{qp}_{hh}"])
                            P.op("dve", lambda e, hh=hh, rz=rz, osb=osb: e.tensor_scalar(out=osb[:, hh, :], in0=bank(3 + hh)[:, 0:64], scalar1=rz[:, hh:hh + 1], scalar2=None, op0=ALU.mult),
                                 reads=[f"bank{3 + hh}", f"rz{qp}_{hh}"], writes=[f"osb{qp}"])
                        P.dma(ODST[t0:t0 + 128, g * 256:(g + 1) * 256], osb.rearrange("p h d -> p (h d)"), reads=[f"osb{qp}"], writes=["ODST"])
            P.barrier()

        if "sel" in stages:
            nsa_branch(1)
        if "win" in stages:
            nsa_branch(2)

        @_stage("diff")
        def _st():
            aF.off, aB.off = attnF0, attnB0
            dk = aB.get([64, 2, S]); dvv = aB.get([128, NQT, 129]); dqs = [aB.get([64, 2, 128]) for _ in range(2)]
            pTs = [aB.get([128, 128]) for _ in range(4)]
            subg = aF.get([128, 128]); rz = aF.get([128, 2]); o1 = aF.get([128, 128]); o2 = aF.get([128, 128])
            sq = aF.get([128, 128]); ss = aF.get([128, 1]); odss = [aF.get([128, 128]) for _ in range(2)]
            P.dma(subg, subg_d.partition_broadcast(128), writes=["subg"])
            P.op("dve", lambda e: e.tensor_scalar(out=subg, in0=subg, scalar1=0.8, scalar2=None, op0=ALU.mult), reads=["subg"], writes=["subg"])
            P.op("pool", lambda e: e.memset(dvv[:, :, 128:129], 1.0), writes=["dv1"])
            for b in range(NB):
                for h in range(4):
                    P.dma(dk, FT[12 + h, :, b * S:(b + 1) * S].rearrange("(m d) t -> d m t", m=2), reads=["FT"], writes=["dk"])
                    P.dma(dvv[:, :, 0:128], DV[b * S:(b + 1) * S, h * 128:(h + 1) * 128].rearrange("(kt p) d -> p kt d", p=128), reads=["DV"], writes=["dvv"])
                    for qt in range(NQT):
                        t0 = b * S + qt * 128
                        qp = qt % 2
                        dq = dqs[qp]; ods = odss[qp]; dqk = f"dq{qp}"
                        P.dma(dq, FT[8 + h, :, t0:t0 + 128].rearrange("(m d) t -> d m t", m=2), reads=["FT"], writes=[dqk])
                        it = 0
                        for m in range(2):
                            for kt in range(qt + 1):
                                bk = it % 4
                                it += 1
                                mm = [(dk[:, m, kt * 128:(kt + 1) * 128], dq[:, m, :], ["dk", dqk])]
                                if kt == qt:
                                    mm.append((identb, bD[:, 8 + h, :], ["identb", "bD"]))
                                elif kt == qt - 1:
                                    mm.append((identb, bP[:, 8 + h, :], ["identb", "bP"]))
                                for mi, (l_, r_, rd) in enumerate(mm):
                                    P.op("pe", lambda e, l_=l_, r_=r_, mi=mi, n=len(mm), bk=bk: e.matmul(out=bank(bk)[:, 0:128], lhsT=l_, rhs=r_, start=(mi == 0), stop=(mi == n - 1)),
                                         reads=rd, writes=[f"bank{bk}"], pe_acc=(mi > 0))
                                P.op("act", lambda e, bk=bk: e.activation(out=pTs[bk], in_=bank(bk)[:, 0:128], func=AF.Exp), reads=[f"bank{bk}"], writes=[f"dpT{bk}"])
                                P.op("pe", lambda e, bk=bk, m=m, kt=kt, qt=qt: e.matmul(out=bank(4 + m)[:, 0:129], lhsT=pTs[bk], rhs=dvv[:, kt, :], start=(kt == 0), stop=(kt == qt)),
                                     reads=[f"dpT{bk}", "dvv", "dv1"], writes=[f"bank{4 + m}"], pe_acc=(kt > 0))
                        P.op("dve", lambda e: e.reciprocal(out=rz[:, 0:1], in_=bank(4)[:, 128:129]), reads=["bank4"], writes=["drz"])
                        P.op("dve", lambda e: e.reciprocal(out=rz[:, 1:2], in_=bank(5)[:, 128:129]), reads=["bank5", "drz"], writes=["drz"])
                        P.op("dve", lambda e: e.tensor_scalar(out=o1, in0=bank(4)[:, 0:128], scalar1=rz[:, 0:1], scalar2=None, op0=ALU.mult), reads=["bank4", "drz"], writes=["o1"])
                        P.op("dve", lambda e: e.tensor_scalar(out=o2, in0=bank(5)[:, 0:128], scalar1=rz[:, 1:2], scalar2=None, op0=ALU.mult), reads=["bank5", "drz"], writes=["o2"])
                        P.op("dve", lambda e: e.scalar_tensor_tensor(out=o1, in0=o2, scalar=neglam[:, 0:1], in1=o1, op0=ALU.mult, op1=ALU.add), reads=["o1", "o2"], writes=["o1"])
                        P.op("act", lambda e: e.activation(out=sq, in_=o1, func=AF.Square, accum_out=ss), reads=["o1"], writes=["dsq", "dss"])
                        P.op("act", lambda e: e.activation(out=ss, in_=ss, func=AF.Sqrt, scale=1.0 / 128, bias=epsb), reads=["dss", "epsb"], writes=["dss"])
                        P.op("dve", lambda e: e.reciprocal(out=ss, in_=ss), reads=["dss"], writes=["dss"])
                        P.op("dve", lambda e, ods=ods: e.scalar_tensor_tensor(out=ods, in0=o1, scalar=ss[:, 0:1], in1=subg, op0=ALU.mult, op1=ALU.mult), reads=["o1", "dss", "subg"], writes=[f"ods{qp}"])
                        P.dma(OD[t0:t0 + 128, h * 128:(h + 1) * 128], ods, reads=[f"ods{qp}"], writes=["OD"])
            P.barrier()

        @_stage("comb")
        def _st():
            aF.reset(); aB.reset()
            wout = aB.get([128, 8, 1024]); wst = aF.get([128, 1024])
            for k in range(8):
                load_cast(wout[:, k, :], wout_d[k], wst, "wout", "wst", eng=("pool", "dve")[k % 2])
            gaB = aF.get([128, 1024])
            oc = aF.get([128, 8, 64]); osx = aF.get([128, 8, 64]); ow = aF.get([128, 8, 64]); gt = aF.get([128, 8, 3])
            mix = aF.get([128, 1024]); mixT = aB.get([128, 8, 128]); xt = aF.get([128, 1024]); x1 = aF.get([128, 1024])
            for b in range(NB):
                P.dma(gaB, MODB[b, 0], reads=["MODB"], writes=["gaB"])
                for qt in range(NQT):
                    t0 = b * S + qt * 128
                    P.dma(oc.rearrange("p h d -> p (h d)"), OC[t0:t0 + 128, :], reads=["OC"], writes=["oc"])
                    P.dma(osx.rearrange("p h d -> p (h d)"), OS[t0:t0 + 128, :], reads=["ODST"], writes=["osx"])
                    P.dma(ow.rearrange("p h d -> p (h d)"), OW[t0:t0 + 128, :], reads=["ODST"], writes=["ow"])
                    P.dma(gt.rearrange("p h c -> p (h c)"), GT[t0:t0 + 128, :], reads=["GT"], writes=["gt"])
                    P.dma(mix[:, 512:1024], OD[t0:t0 + 128, :], reads=["OD"], writes=["mixd"])
                    P.dma(xt, x_d[t0:t0 + 128, :], writes=["cxt"])
                    mv = mix[:, 0:512].rearrange("p (h d) -> p h d", d=64)
                    for (src, gi, key) in ((oc, 0, "oc"), (osx, 1, "osx"), (ow, 2, "ow")):
                        P.op("dve", lambda e, src=src, gi=gi: e.tensor_tensor(out=src, in0=src, in1=gt[:, :, gi:gi + 1].to_broadcast([128, 8, 64]), op=ALU.mult),
                             reads=[key, "gt"], writes=[key])
                    P.op("pool", lambda e: e.tensor_tensor(out=mv, in0=oc, in1=osx, op=ALU.add), reads=["oc", "osx"], writes=["mixn"])
                    P.op("pool", lambda e: e.tensor_tensor(out=mv, in0=mv, in1=ow, op=ALU.add), reads=["mixn", "ow"], writes=["mixn"])
                    for k in range(8):
                        bk = k // 4
                        P.op("pe", lambda e, k=k, bk=bk: e.transpose(out=bank(bk)[:, (k % 4) * 128:(k % 4 + 1) * 128], in_=mix[:, k * 128:(k + 1) * 128], identity=ident),
                             reads=["mixn", "mixd", "ident"], writes=[f"bank{bk}"], pe_acc=(k % 4 > 0))
                    for bk in range(2):
                        evac(mixT[:, bk * 4:(bk + 1) * 4, :].rearrange("p k t -> p (k t)"), bank(bk), [f"bank{bk}"], ["mixT"])
                    for nb2 in range(2):
                        for k in range(8):
                            P.op("pe", lambda e, k=k, nb2=nb2: e.matmul(out=bank(2 + nb2), lhsT=mixT[:, k, :], rhs=wout[:, k, nb2 * 512:(nb2 + 1) * 512], start=(k == 0), stop=(k == 7)),
                                 reads=["mixT", "wout"], writes=[f"bank{2 + nb2}"], pe_acc=(k > 0))
                        P.op("dve", lambda e, nb2=nb2: e.tensor_tensor(out=x1[:, nb2 * 512:(nb2 + 1) * 512], in0=bank(2 + nb2), in1=gaB[:, nb2 * 512:(nb2 + 1) * 512], op=ALU.mult),
                             reads=[f"bank{2 + nb2}", "gaB"], writes=[f"x1_{nb2}"])
                        P.op("pool", lambda e, nb2=nb2: e.tensor_tensor(out=x1[:, nb2 * 512:(nb2 + 1) * 512], in0=x1[:, nb2 * 512:(nb2 + 1) * 512], in1=xt[:, nb2 * 512:(nb2 + 1) * 512], op=ALU.add),
                             reads=[f"x1_{nb2}", "cxt"], writes=[f"x1_{nb2}"])
                    P.dma(X1[t0:t0 + 128, :], x1, reads=["x1_0", "x1_1"], writes=["X1"])
            P.barrier()

        @_stage("peer")
        def _st():
            set_split(15 * 1024)
            NSUB = TP // 128
            wq = aB.get([128, 8, 2048])
            wst = [aF.get([128, 2048]) for _ in range(2)]
            for k in range(8):
                load_cast(wq[:, k, :], wq_d[k], wst[k % 2], "wq", f"wst{k % 2}", eng=("pool", "dve")[k % 2])
            skT = aB.get([128, 2, 128]); skf = aF.get([128, 256])
            load_cast(skT.rearrange("p a b -> p (a b)"), skT_d.rearrange("p a b -> p (a b)"), skf, "skT", "skf", eng="dve")
            P.barrier()
            aF.reset()
            gfB = aF.get([128, 1024]); fingB = aF.get([128, 1024])
            P.dma(fingB, fing_d.partition_broadcast(128), writes=["fingB"])
            x1s = aF.get([128, NSUB, 1024]); xn = aF.get([128, 1024]); scr = aF.get([128, 1024])
            ssq = aF.get([128, NSUB]); rstd = aF.get([128, NSUB])
            scb = aF.get([128, 16, 128]); sv = aF.get([128, 16, 16]); rep = aF.get([128, 128])
            cand = aF.get([128, 8, 256]); rep2 = aF.get([128, 256]); b16 = aF.get([128, 8, 24])
            tau = aF.get([128, 8]); e16 = aF.get([128, 8, 16]); Zp = aF.get([128, 8])
            tmj = aF.get([128, 4, 128])
            trT = aF.get([128, NSUB, 4, 128])
            h2T = aB.get([128, 8, TP]); qT = aB.get([128, 16, TP])
            ActT = aB.get([128, 128, TP])
            GI = 2
            NUB = 4
            ubs = [aB.get([128, GI, 1024]) for _ in range(NUB)]
            qrep = [aB.get([128, 2, 128]) for _ in range(4)]
            Eb = [aF.get([128, 128]) for _ in range(4)]
            Ab = [aB.get([128, 128]) for _ in range(4)]
            Bb = [aB.get([128, 128]) for _ in range(4)]
            ysb = aF.get([128, 1024])
            for b in range(NB):
                P.dma(gfB, MODB[b, 1], reads=["MODB"], writes=["gfB"])
                for tp in range(S // TP):
                    T0 = b * S + tp * TP
                    P.dma(x1s, X1[T0:T0 + TP, :].rearrange("(s p) d -> p s d", p=128), reads=["X1"], writes=["x1s"])
                    rms_rows(x1s, NSUB, ssq, rstd, scr, "x1s", "p2")
                    for s in range(NSUB):
                        P.op("dve", lambda e, s=s: e.tensor_scalar(out=xn, in0=x1s[:, s, :], scalar1=rstd[:, s:s + 1], scalar2=None, op0=ALU.mult),
                             reads=["x1s", "p2rstd"], writes=["pxn"])
                        for k in range(8):
                            bk = k // 4
                            P.op("pe", lambda e, k=k, bk=bk: e.transpose(out=bank(bk)[:, (k % 4) * 128:(k % 4 + 1) * 128], in_=xn[:, k * 128:(k + 1) * 128], identity=ident),
                                 reads=["pxn", "ident"], writes=[f"bank{bk}"], pe_acc=(k % 4 > 0))
                        for k in range(8):
                            bk = k // 4
                            P.op("act", lambda e, k=k, bk=bk, s=s, b=b: e.activation(out=h2T[:, k, s * 128:(s + 1) * 128], in_=bank(bk)[:, (k % 4) * 128:(k % 4 + 1) * 128], func=AF.Identity,
                                                                             scale=A2T[:, k, b:b + 1], bias=B2T[:, k, b:b + 1]),
                                 reads=[f"bank{bk}"], writes=["h2T"])
                    for hp in range(16):
                        bk = 2 + hp % 2
                        for k in range(8):
                            P.op("pe", lambda e, hp=hp, k=k, bk=bk: e.matmul(out=bank(bk)[:, 0:TP], lhsT=wq[:, k, hp * 128:(hp + 1) * 128], rhs=h2T[:, k, :], start=(k == 0), stop=(k == 7)),
                                 reads=["wq", "h2T"], writes=[f"bank{bk}"], pe_acc=(k > 0))
                        evac(qT[:, hp, :], bank(bk)[:, 0:TP], [f"bank{bk}"], ["qT"])
                    for s in range(NSUB):
                        for hp in range(16):
                            bk = 4 + hp // 4
                            P.op("pe", lambda e, hp=hp, bk=bk, s=s: e.matmul(out=bank(bk)[:, (hp % 4) * 128:(hp % 4 + 1) * 128], lhsT=qT[:, hp, s * 128:(s + 1) * 128], rhs=skT[:, hp % 2, :], start=True, stop=True),
                                 reads=["qT", "skT"], writes=[f"bank{bk}"], pe_acc=(hp % 4 > 0))
                        for q4 in range(4):
                            evac(scb[:, q4 * 4:(q4 + 1) * 4, :].rearrange("p a b -> p (a b)"), bank(4 + q4), [f"bank{4 + q4}"], ["scb"])
                        for hp in range(16):
                            P.op("dve", lambda e, hp=hp: e.max(out=sv[:, hp, 0:8], in_=scb[:, hp, :]), reads=["scb"], writes=["sv"])
                            P.op("dve", lambda e, hp=hp: e.match_replace(out=rep, in_to_replace=sv[:, hp, 0:8], in_values=scb[:, hp, :], imm_value=-1e30), reads=["scb", "sv"], writes=["rep"])
                            P.op("dve", lambda e, hp=hp: e.max(out=sv[:, hp, 8:16], in_=rep), reads=["rep", "sv"], writes=["sv"])
                        sv4 = sv.rearrange("p (h two) k -> p h two k", two=2)
                        P.op("dve", lambda e, sv4=sv4: e.tensor_tensor(out=cand.rearrange("p h (a c) -> p h a c", c=16), in0=sv4[:, :, 0, :].unsqueeze(3).to_broadcast([128, 8, 16, 16]),
                                                                in1=sv4[:, :, 1, :].unsqueeze(2).to_broadcast([128, 8, 16, 16]), op=ALU.add), reads=["sv"], writes=["cand"])
                        for h in range(8):
                            P.op("dve", lambda e, h=h: e.max(out=b16[:, h, 0:8], in_=cand[:, h, :]), reads=["cand"], writes=["b16"])
                            P.op("dve", lambda e, h=h: e.match_replace(out=rep2, in_to_replace=b16[:, h, 0:8], in_values=cand[:, h, :], imm_value=-1e30), reads=["cand", "b16"], writes=["rep2"])
                            P.op("dve", lambda e, h=h: e.max(out=b16[:, h, 8:16], in_=rep2), reads=["rep2", "b16"], writes=["b16"])
                            P.op("dve", lambda e, h=h: e.match_replace(out=rep2, in_to_replace=b16[:, h, 8:16], in_values=rep2, imm_value=-1e30), reads=["rep2", "b16"], writes=["rep2"])
                            P.op("dve", lambda e, h=h: e.max(out=b16[:, h, 16:24], in_=rep2), reads=["rep2", "b16"], writes=["b16"])
                        P.op("dve", lambda e: e.tensor_tensor(out=tau, in0=b16[:, :, 15], in1=b16[:, :, 16], op=ALU.add), reads=["b16"], writes=["tau"])
                        P.op("dve", lambda e: e.tensor_scalar(out=tau, in0=tau, scalar1=0.5, scalar2=None, op0=ALU.mult), reads=["tau"], writes=["tau"])
                        P.op("dve", lambda e: e.tensor_tensor(out=e16, in0=b16[:, :, 0:16], in1=b16[:, :, 0:1].to_broadcast([128, 8, 16]), op=ALU.subtract), reads=["b16"], writes=["e16"])
                        P.op("act", lambda e: e.activation(out=e16, in_=e16, func=AF.Exp), reads=["e16"], writes=["e16"])
                        P.op("dve", lambda e: e.tensor_reduce(out=Zp, in_=e16, axis=AX.X, op=ALU.add), reads=["e16"], writes=["Zp"])
                        P.op("dve", lambda e: e.reciprocal(out=Zp, in_=Zp), reads=["Zp"], writes=["Zp"])
                        tm4 = tmj.rearrange("p w (h c) -> p w h c", c=16)
                        sv0 = sv4[:, :, 0, :]; sv1 = sv4[:, :, 1, :]
                        P.op("dve", lambda e, sv1=sv1, tm4=tm4: e.tensor_tensor(out=tm4[:, 0], in0=tau.unsqueeze(2).to_broadcast([128, 8, 16]), in1=sv1, op=ALU.subtract), reads=["tau", "sv"], writes=["tmj"])
                        P.op("dve", lambda e, sv1=sv1, tm4=tm4: e.tensor_tensor(out=tm4[:, 1], in0=sv1, in1=sv1[:, :, 0:1].to_broadcast([128, 8, 16]), op=ALU.subtract), reads=["sv", "tmj"], writes=["tmj"])
                        P.op("act", lambda e, tm4=tm4: e.activation(out=tm4[:, 1], in_=tm4[:, 1], func=AF.Exp), reads=["tmj"], writes=["tmj"])
                        P.op("dve", lambda e, tm4=tm4: e.tensor_tensor(out=tm4[:, 1], in0=tm4[:, 1], in1=Zp.unsqueeze(2).to_broadcast([128, 8, 16]), op=ALU.mult), reads=["tmj", "Zp"], writes=["tmj"])
                        P.op("dve", lambda e, sv0=sv0, tm4=tm4: e.tensor_scalar(out=tm4[:, 2], in0=sv0[:, :, 0:1].to_broadcast([128, 8, 16]), scalar1=-1.0, scalar2=None, op0=ALU.mult), reads=["sv", "tmj"], writes=["tmj"])
                        P.op("dve", lambda e, sv1=sv1, tm4=tm4: e.tensor_copy(out=tm4[:, 3], in_=sv1), reads=["sv", "tmj"], writes=["tmj"])
                        for w in range(4):
                            P.op("pe", lambda e, w=w: e.transpose(out=bank(0)[:, w * 128:(w + 1) * 128], in_=tmj[:, w, :], identity=ident), reads=["tmj", "ident"], writes=["bank0"], pe_acc=(w > 0))
                        P.op("act", lambda e, s=s: e.activation(out=trT[:, s, :, :].rearrange("p w t -> p (w t)"), in_=bank(0), func=AF.Copy), reads=["bank0"], writes=["trT"])
                    for ig in range(128 // GI):
                        r = ig % NUB
                        P.dma(ubs[r], UB[ig * GI:(ig + 1) * GI].rearrange("i p k e -> p i (k e)"), reads=["UBVB"], writes=[f"ubs{r}"])
                        for ii in range(GI):
                            i = ig * GI + ii
                            bk = 1 + i % 2
                            for k in range(8):
                                P.op("pe", lambda e, r=r, ii=ii, k=k, bk=bk: e.matmul(out=bank(bk)[:, 0:TP], lhsT=ubs[r][:, ii, k * 128:(k + 1) * 128], rhs=h2T[:, k, :], start=(k == 0), stop=(k == 7)),
                                     reads=[f"ubs{r}", "h2T"], writes=[f"bank{bk}"], pe_acc=(k > 0))
                            P.op("act", lambda e, i=i, bk=bk: e.activation(out=ActT[:, i, :], in_=bank(bk)[:, 0:TP], func=AF.Gelu_apprx_tanh), reads=[f"bank{bk}"], writes=["ActT"])
                    for t in range(TP):
                        s, tl = t // 128, t % 128
                        r4 = t % 4; r2 = t % 4
                        qv = qT[:, :, t].rearrange("p (h two) -> p h two", two=2)
                        q_in = qv.rearrange("p h two -> p two h").unsqueeze(3).to_broadcast([128, 2, 8, 16])
                        q_out = qrep[r4].rearrange("p two (h c) -> p two h c", c=16)
                        if t % 2 == 0:
                            P.op("pool", lambda e, q_in=q_in, q_out=q_out: e.tensor_copy(out=q_out, in_=q_in), reads=["qT"], writes=[f"qrep{r4}"])
                        else:
                            P.op("act", lambda e, q_in=q_in, q_out=q_out: e.activation(out=q_out, in_=q_in, func=AF.Copy), reads=["qT"], writes=[f"qrep{r4}"])
                        pB = bank(r2)
                        for p_ in range(2):
                            P.op("pe", lambda e, r4=r4, p_=p_, pB=pB: e.matmul(out=pB[:, p_ * 128:(p_ + 1) * 128], lhsT=qrep[r4][:, p_, :], rhs=skT[:, p_, :], start=True, stop=True),
                                 reads=[f"qrep{r4}", "skT"], writes=[f"bank{r2}"], pe_acc=(p_ > 0))
                        P.op("act", lambda e, r2=r2, pB=pB, s=s, tl=tl: e.activation(out=Eb[r2], in_=pB[:, 0:128], func=AF.Exp, bias=trT[:, s, 2, tl:tl + 1]),
                             reads=[f"bank{r2}", "trT"], writes=[f"Eb{r2}"])
                        P.op("dve", lambda e, r2=r2, pB=pB, s=s, tl=tl: e.scalar_tensor_tensor(out=Ab[r2], in0=pB[:, 0:128], scalar=trT[:, s, 0, tl:tl + 1], in1=Eb[r2], op0=ALU.is_ge, op1=ALU.mult),
                             reads=[f"bank{r2}", "trT", f"Eb{r2}"], writes=[f"Ab{r2}"])
                        P.op("dve", lambda e, r2=r2, pB=pB, s=s, tl=tl: e.tensor_scalar(out=Bb[r2], in0=pB[:, 128:256], scalar1=trT[:, s, 3, tl:tl + 1], scalar2=trT[:, s, 1, tl:tl + 1], op0=ALU.is_equal, op1=ALU.mult),
                             reads=[f"bank{r2}", "trT"], writes=[f"Bb{r2}"])
                        pW = bank(4 + r2)
                        P.op("pe", lambda e, r2=r2, pW=pW: e.matmul(out=pW[:, 0:128], lhsT=Bb[r2], rhs=Ab[r2], start=True, stop=True),
                             reads=[f"Ab{r2}", f"Bb{r2}"], writes=[f"bank{4 + r2}"])
                        if t % 3 == 0:
                            P.op("act", lambda e, r2=r2, pW=pW: e.activation(out=Eb[r2], in_=pW[:, 0:128], func=AF.Copy), reads=[f"bank{4 + r2}", f"Ab{r2}"], writes=[f"Eb{r2}"])
                            P.op("pool", lambda e, r2=r2, t=t: e.tensor_tensor(out=ActT[:, :, t], in0=ActT[:, :, t], in1=Eb[r2], op=ALU.mult), reads=[f"Eb{r2}", "ActT"], writes=[f"WA{t}"])
                        else:
                            P.op("dve", lambda e, pW=pW, t=t: e.tensor_tensor(out=ActT[:, :, t], in0=pW[:, 0:128], in1=ActT[:, :, t], op=ALU.mult), reads=[f"bank{4 + r2}", "ActT"], writes=[f"WA{t}"])
                    wak = [f"WA{t}" for t in range(TP)]
                    for s in range(NSUB):
                        for half in range(128 // GI // 1):
                            pass
                    nV = 0
                    for ig in range(128 // GI):
                        r = ig % NUB
                        P.dma(ubs[r], VB[ig * GI:(ig + 1) * GI].rearrange("i p n -> p i n"), reads=["UBVB"], writes=[f"ubs{r}"])
                        for ii in range(GI):
                            i = ig * GI + ii
                            for s in range(NSUB):
                                for nb2 in range(2):
                                    bk = s * 2 + nb2 if NSUB <= 2 else None
                                    P.op("pe", lambda e, r=r, ii=ii, i=i, s=s, nb2=nb2, bk=bk: e.matmul(out=bank(bk), lhsT=ActT[:, i, s * 128:(s + 1) * 128], rhs=ubs[r][:, ii, nb2 * 512:(nb2 + 1) * 512], start=(i == 0), stop=(i == 127)),
                                         reads=[f"ubs{r}", "ActT"] + (wak if i == 0 else []), writes=[f"bank{bk}"], pe_acc=(i > 0))
                    for s in range(NSUB):
                        for nb2 in range(2):
                            bk = s * 2 + nb2
                            P.op("dve", lambda e, nb2=nb2, bk=bk: e.tensor_tensor(out=ysb[:, nb2 * 512:(nb2 + 1) * 512], in0=bank(bk), in1=gfB[:, nb2 * 512:(nb2 + 1) * 512], op=ALU.mult),
                                 reads=[f"bank{bk}", "gfB"], writes=["ysb"])
                        P.op("dve", lambda e, s=s: e.tensor_tensor(out=ysb, in0=ysb, in1=x1s[:, s, :], op=ALU.add), reads=["ysb", "x1s"], writes=["ysb"])
                        P.op("act", lambda e, s=s: e.activation(out=scr, in_=ysb, func=AF.Square, accum_out=ssq[:, 0:1]), reads=["ysb"], writes=["p2scr", "p2ssq"])
                        P.op("act", lambda e: e.activation(out=rstd[:, 0:1], in_=ssq[:, 0:1], func=AF.Sqrt, scale=1.0 / D, bias=epsb), reads=["p2ssq", "epsb"], writes=["p2rstd"])
                        P.op("dve", lambda e: e.reciprocal(out=rstd[:, 0:1], in_=rstd[:, 0:1]), reads=["p2rstd"], writes=["p2rstd"])
                        P.op("dve", lambda e: e.scalar_tensor_tensor(out=xn, in0=ysb, scalar=rstd[:, 0:1], in1=fingB, op0=ALU.mult, op1=ALU.mult), reads=["ysb", "p2rstd", "fingB"], writes=["pxn"])
                        P.dma(out_d[T0 + s * 128:T0 + (s + 1) * 128, :], xn, reads=["pxn"], writes=["OUT"])
            P.barrier()

        P.barrier()
        P.replay()
    return nc


def make_in_maps(inputs, n_cores, NB, S):
    f = lambda a: np.ascontiguousarray(np.asarray(a, dtype=np.float32))
    x = f(inputs["x"]); c = f(inputs["c"])
    tabs = host_tables(S, f(inputs["rel_bias"]))
    perm = win_perm()
    shared = {
        "ada_w": f(inputs["ada_w"][0]).reshape(8, 128, 6144),
        "ada_bT": f(f(inputs["ada_b"][0]).reshape(48, 128).T),
        "ada_b": f(inputs["ada_b"][0]).reshape(1, 6144),
        "g12T": f(np.stack([f(inputs["norm1_g"][0]).reshape(8, 128).T, f(inputs["norm2_g"][0]).reshape(8, 128).T], axis=1)),
        "w_in": f(f(inputs["w_in"][0])[:, perm]).reshape(8, 128, 2840),
        "w_out": f(inputs["w_out"][0]).reshape(8, 128, 1024),
        "posT": f(f(inputs["cmp_pos"][0]).transpose(2, 0, 1)),
        "w1r": f(f(inputs["cmp_w1"][0]).reshape(2, 32, 64, 256).transpose(0, 2, 1, 3)),
        "w2r": f(f(inputs["cmp_w2"][0]).reshape(2, 2, 128, 64).transpose(2, 0, 1, 3)),
        "lamv": f(np.stack([inputs["lam_q1"][0], inputs["lam_k1"][0], inputs["lam_q2"][0], inputs["lam_k2"][0]])),
        "subg": f(inputs["diff_subln_g"][0]).reshape(1, 128),
        "wq": f(inputs["peer_wq"][0]).reshape(8, 128, 2048),
        "skT": f(f(inputs["peer_sub_keys"][0]).transpose(2, 0, 1)),
        "uT": f(f(inputs["peer_u"][0]).reshape(128, 128, 8, 128).transpose(0, 3, 2, 1)),
        "v": f(inputs["peer_v"][0]).reshape(128, 128, 1024),
        "fing": f(inputs["final_g"]).reshape(1, 1024),
    }
    shared.update(tabs)
    maps = []
    for i in range(n_cores):
        xs = x[i * NB:(i + 1) * NB].reshape(NB * S, D)
        cs = c[i * NB:(i + 1) * NB]
        cT = f(cs.reshape(NB, 8, 128).transpose(2, 1, 0))
        m = dict(shared)
        m["x"] = f(xs)
        m["cT"] = cT
        maps.append(m)
    return maps


def kernel(**inputs):
    B, S, _ = inputs["x"].shape
    n_cores = 8
    NB = B // n_cores
    nc = build(NB, S)
    in_maps = make_in_maps(inputs, n_cores, NB, S)
    res = run_bass_kernel_spmd(nc, in_maps, core_ids=list(range(n_cores)))
    outs = [np.asarray(r["out"]).reshape(NB, S, D) for r in res.results]
    return np.concatenate(outs, axis=0).astype(np.float32)
```

```python
import math
from contextlib import ExitStack
import numpy as np
import concourse.bass as bass
import concourse.mybir as mybir
from concourse.bass_utils import run_bass_kernel_spmd

F32 = mybir.dt.float32
BF = mybir.dt.bfloat16
ALU = mybir.AluOpType
AF = mybir.ActivationFunctionType
AX = mybir.AxisListType

D = 1024
NEG = -30000.0
EPOCH = 16000
NDMA_SEMS = 24


class Planner:
    ENGS = ("pe", "act", "dve", "pool", "sp")

    def __init__(self, nc, stack):
        self.nc = nc
        self.stack = stack
        self.items = {e: [] for e in self.ENGS}
        self.seq = {e: 0 for e in self.ENGS}
        self.esems = {e: [] for e in self.ENGS}
        self.waited = {e: {} for e in self.ENGS}
        self.semobjs = []
        self.last_w = {}
        self.readers = {}
        self.dma_sems = []
        self.dma_cum = []
        self.dma_next = 0
        self.last_ev = {}
        self.marks = []

    def mark(self, name):
        self.marks.append((name, dict(self.seq)))

    def _newsem(self, name):
        s = self.stack.enter_context(self.nc.semaphore(name))
        self.semobjs.append(s)
        return len(self.semobjs) - 1

    def _eng_event(self, e):
        n = self.seq[e]
        self.seq[e] += 1
        ep = n // EPOCH
        while len(self.esems[e]) <= ep:
            self.esems[e].append(self._newsem(f"s_{e}_{len(self.esems[e])}"))
        return (self.esems[e][ep], n % EPOCH + 1)

    def _dma_event(self, extra):
        if len(self.dma_sems) < NDMA_SEMS:
            self.dma_sems.append(self._newsem(f"s_dma_{len(self.dma_sems)}"))
            self.dma_cum.append(0)
        i = self.dma_next % NDMA_SEMS
        self.dma_next += 1
        if self.dma_cum[i] > 0:
            extra.append((self.dma_sems[i], self.dma_cum[i]))
        self.dma_cum[i] += 16
        return (self.dma_sems[i], self.dma_cum[i])

    def _deps(self, reads, writes):
        evs = []
        for k in reads:
            w = self.last_w.get(k)
            if w is not None:
                evs.append(w)
        for k in writes:
            w = self.last_w.get(k)
            if w is not None:
                evs.append(w)
            evs.extend(self.readers.get(k, ()))
        return evs

    def _commit(self, ev, reads, writes):
        for k in reads:
            self.readers.setdefault(k, []).append(ev)
        for k in writes:
            self.last_w[k] = ev
            self.readers[k] = []

    def _filter(self, e, evs):
        best = {}
        for (s, v) in evs:
            if v > best.get(s, 0):
                best[s] = v
        out = []
        wd = self.waited[e]
        for s, v in best.items():
            if wd.get(s, 0) >= v:
                continue
            wd[s] = v
            out.append((s, v))
        return out

    def op(self, e, fn, reads=(), writes=(), pe_acc=False):
        evs = self._deps(reads, writes)
        ev = self._eng_event(e)
        if pe_acc:
            own = set(self.esems[e])
            wk = set(writes)
            evs2 = []
            for k in reads:
                w = self.last_w.get(k)
                if w is not None:
                    evs2.append(w)
            for k in wk:
                w = self.last_w.get(k)
                if w is not None and w[0] not in own:
                    evs2.append(w)
                evs2.extend(self.readers.get(k, ()))
            evs = evs2
        waits = self._filter(e, evs)
        self.items[e].append((waits, fn, (ev[0], 1)))
        self._commit(ev, reads, writes)
        self.last_ev[e] = ev

    def dma(self, out_ap, in_ap, reads=(), writes=(), e="sp"):
        evs = self._deps(reads, writes)
        extra = []
        ev = self._dma_event(extra)
        waits = self._filter(e, evs + extra)

        def fn(eng, out_ap=out_ap, in_ap=in_ap):
            return eng.dma_start(out=out_ap, in_=in_ap)
        self.items[e].append((waits, fn, (ev[0], 16)))
        self._commit(ev, reads, writes)

    def barrier(self):
        evs = [(s, c) for s, c in zip(self.dma_sems, self.dma_cum) if c > 0]
        evs += list(self.last_ev.values())
        for e in self.ENGS:
            waits = self._filter(e, evs)
            if waits:
                self.items[e].append((waits, None, None))
        self.last_w = {}
        self.readers = {}

    def replay(self):
        nc = self.nc
        sem = self.semobjs

        def run(eng, items):
            for waits, fn, inc in items:
                for (s, v) in waits:
                    eng.wait_ge(sem[s], v)
                if fn is None:
                    continue
                ins = fn(eng)
                ins.then_inc(sem[inc[0]], inc[1])

        with nc.Block() as block:
            @block.tensor
            def _(eng):
                run(eng, self.items["pe"])

            @block.scalar
            def _(eng):
                run(eng, self.items["act"])

            @block.vector
            def _(eng):
                run(eng, self.items["dve"])

            @block.gpsimd
            def _(eng):
                run(eng, self.items["pool"])

            @block.sync
            def _(eng):
                run(eng, self.items["sp"])


class Arena:
    def __init__(self, t2d):
        self.t = t2d
        self.off = 0
        self.n = t2d.shape[1]

    def reset(self):
        self.off = 0

    def get(self, shape):
        p = shape[0]
        n = int(np.prod(shape[1:]))
        assert self.off + n <= self.n, ("arena overflow", self.off, n, self.n)
        ap = self.t[0:p, self.off:self.off + n]
        self.off += n
        if len(shape) == 3:
            ap = ap.rearrange("p (a b) -> p a b", a=shape[1], b=shape[2])
        elif len(shape) == 4:
            ap = ap.rearrange("p (a b c) -> p a b c", a=shape[1], b=shape[2], c=shape[3])
        return ap


def t5_bucket_np(dist):
    n = np.maximum(dist, 0)
    nf = np.maximum(n, 1).astype(np.float32)
    large = 16 + (np.log(nf / np.float32(16)) / np.float32(math.log(128 / 16)) * np.float32(16)).astype(np.int32)
    return np.where(n < 16, n, np.minimum(large, 31)).astype(np.int64)


def t5_bucket_jax(dist):
    import jax.numpy as jnp
    dist = jnp.asarray(dist, jnp.int32)
    n = jnp.maximum(dist, 0)
    nf = jnp.maximum(n, 1).astype(jnp.float32)
    large = 16 + (jnp.log(nf / 16) / math.log(128 / 16) * 16).astype(jnp.int32)
    return np.asarray(jnp.where(n < 16, n, jnp.minimum(large, 31))).astype(np.int64)


def host_tables(S, rel_bias):
    bucket = t5_bucket_np
    kr = np.arange(128)[:, None]
    tr = np.arange(128)[None, :]
    d_diag = tr - kr
    d_prev = tr - kr + 128
    bk_diag = bucket(np.maximum(d_diag, 0))
    bk_prev = bucket(d_prev)
    rb = rel_bias.astype(np.float32)
    t = {}
    t["gD"] = np.ascontiguousarray(rb[bk_diag].transpose(0, 2, 1))
    t["gP"] = np.ascontiguousarray(rb[bk_prev].transpose(0, 2, 1))
    t["mD"] = np.where(d_diag >= 0, 0.0, NEG).astype(np.float32)
    t["mW"] = np.where(kr > tr, 0.0, NEG).astype(np.float32)
    trc = np.arange(128)[:, None]
    cr = np.arange(16)[None, :]
    d_c = trc - 16 * cr + 113
    t["gC"] = np.ascontiguousarray(rb[bucket(np.maximum(d_c, 0))][:, :, :8].transpose(0, 2, 1))
    t["mC"] = np.where(d_c >= 0, 0.0, NEG).astype(np.float32)
    t["r31"] = np.ascontiguousarray(np.broadcast_to(rb[31][None, :], (128, 12)))
    nsel = S // 64
    nqt = S // 128
    tt = np.arange(S)
    cur = tt // 64
    j = np.arange(64 if nsel >= 64 else nsel)
    NJ = 64
    lo = np.full((S, NJ), -1e30, np.float32)
    hi = np.full((S, NJ), 3e38, np.float32)
    jj = np.arange(NJ)[None, :]
    c2 = cur[:, None]
    lo[(jj == c2 - 1) & (jj < nsel)] = 1e9
    lo[(jj == c2) & (jj < nsel)] = 2e9
    lo[np.broadcast_to(jj == 0, lo.shape)] = 3e9
    hi[np.broadcast_to(jj, hi.shape) > c2] = -1e30
    t["LO"] = np.ascontiguousarray(lo.reshape(nqt, 128, NJ).transpose(1, 0, 2))
    t["HI"] = np.ascontiguousarray(hi.reshape(nqt, 128, NJ).transpose(1, 0, 2))
    ncmp = (S - 32) // 16 + 1
    cs = np.arange(ncmp) * 16
    ss = np.arange(nsel) * 64
    lo_ = np.maximum(cs[:, None], ss[None, :])
    hi_ = np.minimum(cs[:, None] + 32, ss[None, :] + 64)
    c2s = np.zeros((256, NJ), np.float32)
    c2s[:ncmp, :nsel] = np.maximum(hi_ - lo_, 0).astype(np.float32) / 32
    t["C2S"] = np.ascontiguousarray(c2s.reshape(2, 128, NJ).transpose(1, 0, 2))
    w2 = np.zeros((64, 64, 64), np.float32)
    for q in range(64):
        w2[q, q, :] = 1.0
    t["W2"] = w2.reshape(64, 4096)
    return t


def win_perm():
    cols = []
    cols += list(range(0, 512))
    kv = 512
    cols += list(range(kv + 0, kv + 128))
    cols += list(range(kv + 128, kv + 256))
    cols += list(range(kv + 256, kv + 384))
    cols += list(range(kv + 512, kv + 640))
    cols += list(range(1304, 1816))
    cols += list(range(1816, 2328))
    cols += list(range(kv + 384, kv + 512))
    cols += list(range(kv + 640, kv + 768))
    cols += list(range(1280, 1304))
    cols += list(range(2328, 2840))
    assert len(cols) == 2840 and len(set(cols)) == 2840
    return np.array(cols)


def build(NB, S, debug=(), stages=None, TP=256):
    nc = bass.Bass("TRN2", target_bir_lowering=False)
    T = NB * S
    NQT = S // 128
    NBLK = S // 512
    C = S // 16 - 1
    NCT = (C + 127) // 128
    all_stages = ["pre", "mod", "proj", "cmp", "csel", "sel", "win", "diff", "comb", "peer"]
    stages = all_stages if stages is None else stages

    def din(name, shape, dt=F32):
        return nc.dram_tensor(name, list(shape), dt, kind="ExternalInput").ap()

    def dscr(name, shape, dt=F32):
        kind = "ExternalOutput" if name in debug else "Internal"
        return nc.dram_tensor(name, list(shape), dt, kind=kind).ap()

    x_d = din("x", [T, D])
    cT_d = din("cT", [128, 8, NB])
    adaw_d = din("ada_w", [8, 128, 6144])
    adabT_d = din("ada_bT", [128, 48])
    adab_d = din("ada_b", [1, 6144])
    g12T_d = din("g12T", [128, 2, 8])
    win_d = din("w_in", [8, 128, 2840])
    wout_d = din("w_out", [8, 128, 1024])
    posT_d = din("posT", [64, 2, 32])
    w1_d = din("w1r", [2, 64, 32, 256])
    w2_d = din("w2r", [128, 2, 2, 64])
    lam_d = din("lamv", [4, 64])
    subg_d = din("subg", [1, 128])
    wq_d = din("wq", [8, 128, 2048])
    skT_d = din("skT", [128, 2, 128])
    uT_d = din("uT", [128, 128, 8, 128])
    v_d = din("v", [128, 128, 1024])
    fing_d = din("fing", [1, 1024])
    gD_d = din("gD", [128, 12, 128]); gP_d = din("gP", [128, 12, 128])
    mD_d = din("mD", [128, 128]); mW_d = din("mW", [128, 128])
    gC_d = din("gC", [128, 8, 16]); mC_d = din("mC", [128, 16]); r31_d = din("r31", [128, 12])
    LO_d = din("LO", [128, NQT, 64]); HI_d = din("HI", [128, NQT, 64])
    C2S_d = din("C2S", [128, 2, 64]); W2_d = din("W2", [64, 4096])
    out_d = nc.dram_tensor("out", [T, D], F32, kind="ExternalOutput").ap()

    FT = dscr("FT", [16, 128, T], BF)
    TMV = dscr("TMV", [T, 256], BF)
    GT = dscr("GT", [T, 24])
    DV = dscr("DV", [T, 512], BF)
    KC = dscr("KC", [NB, 2, 64, 256], BF)
    VC = dscr("VC", [NB, 2, 256, 64])
    OC = dscr("OC", [T, 512]); OS = dscr("OS", [T, 512]); OW = dscr("OW", [T, 512]); OD = dscr("OD", [T, 512])
    NM = dscr("NM", [NB, 2, 64, S], BF)
    X1 = dscr("X1", [T, D])
    UB = dscr("UB", [128, 128, 8, 128], BF)
    VB = dscr("VB", [128, 128, 1024], BF)
    MODD = dscr("MODD", [128, 64, NB])

    with ExitStack() as st:
        P = Planner(nc, st)
        NF = 22 * 1024
        NBF = 52 * 1024
        NU = NF + NBF // 2
        arU_t = st.enter_context(nc.sbuf_tensor("arU", [128, NU], F32))
        pers_t = st.enter_context(nc.sbuf_tensor("pers", [128, 512], F32))
        persb_t = st.enter_context(nc.sbuf_tensor("persb", [128, 256], BF))
        psum_t = st.enter_context(nc.psum_tensor("psum", [128, 4096], F32))
        aF = Arena(arU_t[:, 0:NF]); aB = Arena(arU_t[:, NF:NU].bitcast(BF)); aP = Arena(pers_t[:]); aPb = Arena(persb_t[:])

        def set_split(nf):
            aF.t = arU_t[:, 0:nf]; aF.n = nf; aF.off = 0
            aB.t = arU_t[:, nf:NU].bitcast(BF); aB.n = (NU - nf) * 2; aB.off = 0

        def bank(i, n=512):
            return psum_t[:, i * 512:i * 512 + n]

        def _stage(name):
            def deco(f):
                if name in stages:
                    f()
                return f
            return deco

        ident = aP.get([128, 128]); epsb = aP.get([128, 1])
        A1T = aP.get([128, 8, NB]); B1T = aP.get([128, 8, NB]); A2T = aP.get([128, 8, NB]); B2T = aP.get([128, 8, NB])
        neglam = aP.get([128, 1])
        identb = aPb.get([128, 128])
        MODB = dscr("MODB", [NB, 2, 128, 1024])

        P.op("pool", lambda e: e.memset(ident, 1.0), writes=["ident"])
        P.op("pool", lambda e: e.affine_select(out=ident, in_=ident, pattern=[[-1, 128]], compare_op=ALU.is_equal,
                                               fill=0.0, base=0, channel_multiplier=1), reads=["ident"], writes=["ident"])
        P.op("pool", lambda e: e.memset(epsb, 1e-6), writes=["epsb"])
        P.op("dve", lambda e: e.tensor_copy(out=identb, in_=ident), reads=["ident"], writes=["identb"])

        evac_rr = [0]

        def evac(out, in_, reads, writes, scale=None):
            evac_rr[0] ^= 1
            if evac_rr[0]:
                if scale is None:
                    P.op("act", lambda e: e.activation(out=out, in_=in_, func=AF.Copy), reads=reads, writes=writes)
                else:
                    P.op("act", lambda e: e.activation(out=out, in_=in_, func=AF.Copy, scale=scale), reads=reads, writes=writes)
            else:
                if scale is None:
                    P.op("dve", lambda e: e.tensor_copy(out=out, in_=in_), reads=reads, writes=writes)
                else:
                    P.op("dve", lambda e: e.tensor_scalar(out=out, in0=in_, scalar1=scale, scalar2=None, op0=ALU.mult),
                         reads=reads, writes=writes)

        def load_cast(dst_bf, src_dram, stage_f32, key_dst, key_stage, eng="pool"):
            P.dma(stage_f32, src_dram, writes=[key_stage])
            P.op(eng, lambda e: e.tensor_copy(out=dst_bf, in_=stage_f32), reads=[key_stage], writes=[key_dst])

        def rms_rows(xt, nsub, ssq, rstd, scr, kx, tag):
            for s in range(nsub):
                P.op("act", lambda e, s=s: e.activation(out=scr, in_=xt[:, s, :], func=AF.Square, accum_out=ssq[:, s:s + 1]),
                     reads=[kx], writes=[tag + "scr", tag + "ssq"])
            P.op("act", lambda e: e.activation(out=rstd, in_=ssq, func=AF.Sqrt, scale=1.0 / D, bias=epsb),
                 reads=[tag + "ssq", "epsb"], writes=[tag + "rstd"])
            P.op("dve", lambda e: e.reciprocal(out=rstd, in_=rstd), reads=[tag + "rstd"], writes=[tag + "rstd"])

        @_stage("pre")
        def _st():
            aF.reset(); aB.reset()
            G = 4
            stg = [aF.get([128, G, 1024]) for _ in range(2)]
            stb = [aB.get([128, G, 1024]) for _ in range(2)]
            it = 0
            for (src, dst, is_u) in ((uT_d, UB, True), (v_d, VB, False)):
                for ig in range(128 // G):
                    r = it % 2
                    it += 1
                    if is_u:
                        s_ap = src[ig * G:(ig + 1) * G].rearrange("i p k e -> p i (k e)")
                        d_ap = dst[ig * G:(ig + 1) * G].rearrange("i p k e -> p i (k e)")
                    else:
                        s_ap = src[ig * G:(ig + 1) * G].rearrange("i p n -> p i n")
                        d_ap = dst[ig * G:(ig + 1) * G].rearrange("i p n -> p i n")
                    P.dma(stg[r], s_ap, writes=[f"stg{r}"])
                    eng = ("pool", "dve", "act")[it % 3]
                    if eng == "act":
                        P.op("act", lambda e, r=r: e.activation(out=stb[r], in_=stg[r], func=AF.Copy), reads=[f"stg{r}"], writes=[f"stb{r}"])
                    else:
                        P.op(eng, lambda e, r=r: e.tensor_copy(out=stb[r], in_=stg[r]), reads=[f"stg{r}"], writes=[f"stb{r}"])
                    P.dma(d_ap, stb[r], reads=[f"stb{r}"], writes=["UBVB"])
            P.barrier()

        @_stage("mod")
        def _st():
            aF.reset(); aB.reset()
            condT = aF.get([128, 8, NB])
            crep = aF.get([128, 8 * NB, 128])
            adab_T = aF.get([128, 48])
            g12T = aF.get([128, 2, 8])
            modT = aF.get([128, 48, NB])
            adab_bc = aF.get([128, 2, 1024])
            wall = aF.get([128, 8, 2048])
            gout = aF.get([128, 1024])
            P.dma(condT, cT_d, writes=["condT"])
            P.dma(adab_T, adabT_d, writes=["adabT"])
            P.dma(g12T, g12T_d, writes=["g12T"])
            P.dma(adab_bc[:, 0, :], adab_d[:, 2048:3072].partition_broadcast(128), writes=["adab_bc"])
            P.dma(adab_bc[:, 1, :], adab_d[:, 5120:6144].partition_broadcast(128), writes=["adab_bc"])
            P.op("act", lambda e: e.activation(out=condT, in_=condT, func=AF.Silu), reads=["condT"], writes=["condT"])
            P.op("dve", lambda e: e.tensor_copy(out=crep, in_=condT.rearrange("p k b -> p (k b)").unsqueeze(2).to_broadcast([128, 8 * NB, 128])),
                 reads=["condT"], writes=["crep"])
            pm = bank(0)
            mlist = list(range(0, 16)) + list(range(24, 40))
            for half, cols0 in ((0, 0), (1, 3072)):
                for k in range(8):
                    P.dma(wall[:, k, :], adaw_d[k, :, cols0:cols0 + 2048], writes=["wall"])
                for mi in range(16):
                    m = (0 if half == 0 else 24) + mi
                    for k in range(8):
                        P.op("pe", lambda e, mi=mi, m=m, k=k: e.matmul(out=pm[:, m * NB:(m + 1) * NB], lhsT=wall[:, k, mi * 128:(mi + 1) * 128],
                                                                 rhs=condT[:, k, :], start=(k == 0), stop=(k == 7)),
                             reads=["wall", "condT"], writes=["bank0"], pe_acc=(k > 0))
            P.op("dve", lambda e: e.tensor_tensor(out=modT, in0=pm[:, 0:48 * NB].rearrange("p (m b) -> p m b", b=NB),
                                                  in1=adab_T.unsqueeze(2).to_broadcast([128, 48, NB]), op=ALU.add),
                 reads=["bank0", "adabT"], writes=["modT"])
            for (At, Bt, gi, msh, msc) in ((A1T, B1T, 0, 0, 8), (A2T, B2T, 1, 24, 32)):
                P.op("dve", lambda e, At=At, msc=msc: e.tensor_scalar(out=At, in0=modT[:, msc:msc + 8, :], scalar1=1.0, scalar2=None, op0=ALU.add),
                     reads=["modT"], writes=["AB"])
                P.op("dve", lambda e, At=At, gi=gi: e.tensor_tensor(out=At, in0=At, in1=g12T[:, gi, :].unsqueeze(2).to_broadcast([128, 8, NB]), op=ALU.mult),
                     reads=["AB", "g12T"], writes=["AB"])
                P.op("dve", lambda e, Bt=Bt, msh=msh: e.tensor_copy(out=Bt, in_=modT[:, msh:msh + 8, :]), reads=["modT"], writes=["AB"])
            for b in range(NB):
                for gi, c0 in ((0, 2048), (1, 5120)):
                    for k in range(8):
                        P.dma(wall[:, k, 0:1024], adaw_d[k, :, c0:c0 + 1024], writes=["wall"])
                    for nb2 in range(2):
                        for k in range(8):
                            P.op("pe", lambda e, nb2=nb2, k=k, b=b: e.matmul(out=bank(1 + nb2), lhsT=crep[:, k * NB + b, :],
                                                                      rhs=wall[:, k, nb2 * 512:(nb2 + 1) * 512], start=(k == 0), stop=(k == 7)),
                                 reads=["wall", "crep"], writes=[f"bank{1 + nb2}"], pe_acc=(k > 0))
                    for nb2 in range(2):
                        P.op("dve", lambda e, nb2=nb2, gi=gi: e.tensor_tensor(out=gout[:, nb2 * 512:(nb2 + 1) * 512], in0=bank(1 + nb2),
                                                                       in1=adab_bc[:, gi, nb2 * 512:(nb2 + 1) * 512], op=ALU.add),
                             reads=[f"bank{1 + nb2}", "adab_bc"], writes=["gout"])
                    P.dma(MODB[b, gi], gout, reads=["gout"], writes=["MODB"])
            if "MODD" in debug:
                P.dma(MODD[:, 0:48, :], modT, reads=["modT"], writes=["MODD"])
            lamt = aF.get([128, 4, 64]); lprod = aF.get([128, 2, 64]); lsum = aF.get([128, 2])
            P.dma(lamt.rearrange("p a b -> p (a b)"), lam_d.rearrange("a b -> (a b)").unsqueeze(0).partition_broadcast(128) if False else
                  lam_d.rearrange("(o a) b -> o (a b)", o=1).partition_broadcast(128), writes=["lamt"])
            P.op("dve", lambda e: e.tensor_tensor(out=lprod[:, 0, :], in0=lamt[:, 0, :], in1=lamt[:, 1, :], op=ALU.mult), reads=["lamt"], writes=["lprod"])
            P.op("dve", lambda e: e.tensor_tensor(out=lprod[:, 1, :], in0=lamt[:, 2, :], in1=lamt[:, 3, :], op=ALU.mult), reads=["lprod", "lamt"], writes=["lprod"])
            P.op("dve", lambda e: e.tensor_reduce(out=lsum, in_=lprod, axis=AX.X, op=ALU.add), reads=["lprod"], writes=["lsum"])
            P.op("act", lambda e: e.activation(out=lsum, in_=lsum, func=AF.Exp), reads=["lsum"], writes=["lsum"])
            P.op("dve", lambda e: e.tensor_tensor(out=neglam, in0=lsum[:, 1:2], in1=lsum[:, 0:1], op=ALU.subtract), reads=["lsum"], writes=["neglam"])
            P.op("dve", lambda e: e.tensor_scalar(out=neglam, in0=neglam, scalar1=-0.2, scalar2=None, op0=ALU.add), reads=["neglam"], writes=["neglam"])
            P.barrier()

        @_stage("proj")
        def _st():
            aF.reset(); aB.reset()
            win = aB.get([128, 8, 2840])
            wst = [aF.get([128, 2840]) for _ in range(2)]
            for k in range(8):
                load_cast(win[:, k, :], win_d[k], wst[k % 2], "win", f"wst{k % 2}", eng=("pool", "dve")[k % 2])
            xt = aF.get([128, 4, 1024]); xn = aF.get([128, 4, 1024]); scr = aF.get([128, 1024])
            ssq = aF.get([128, 4]); rstd = aF.get([128, 4])
            hT = aB.get([128, 8, 512])
            ftsb = aB.get([128, 16, 512])
            tmv = aB.get([128, 4, 256]); dvs = aB.get([128, 4, 512])
            gts = aF.get([128, 4, 24])
            for b in range(NB):
                for blk in range(NBLK):
                    r0 = b * S + blk * 512
                    P.dma(xt, x_d[r0:r0 + 512, :].rearrange("(s p) d -> p s d", p=128), writes=["xt"])
                    rms_rows(xt, 4, ssq, rstd, scr, "xt", "p1")
                    for s in range(4):
                        P.op("dve", lambda e, s=s: e.tensor_scalar(out=xn[:, s, :], in0=xt[:, s, :], scalar1=rstd[:, s:s + 1], scalar2=None, op0=ALU.mult),
                             reads=["xt", "p1rstd"], writes=[f"xn{s}"])
                    for k in range(8):
                        bk = k % 2
                        for s in range(4):
                            P.op("pe", lambda e, s=s, k=k, bk=bk: e.transpose(out=bank(bk)[:, s * 128:(s + 1) * 128], in_=xn[:, s, k * 128:(k + 1) * 128], identity=ident),
                                 reads=[f"xn{s}", "ident"], writes=[f"bank{bk}"], pe_acc=(s > 0))
                        P.op("act", lambda e, k=k, bk=bk, b=b: e.activation(out=hT[:, k, :], in_=bank(bk), func=AF.Identity, scale=A1T[:, k, b:b + 1], bias=B1T[:, k, b:b + 1]),
                             reads=[f"bank{bk}"], writes=[f"hT{k}"])
                    hkeys = [f"hT{k}" for k in range(8)]
                    for c in range(16):
                        bk = 2 + c % 3
                        for k in range(8):
                            P.op("pe", lambda e, c=c, k=k, bk=bk: e.matmul(out=bank(bk), lhsT=win[:, k, c * 128:(c + 1) * 128], rhs=hT[:, k, :], start=(k == 0), stop=(k == 7)),
                                 reads=["win"] + hkeys, writes=[f"bank{bk}"], pe_acc=(k > 0))
                        sc_ = 0.125 if (c < 4 or 8 <= c < 12) else None
                        evac(ftsb[:, c, :], bank(bk), [f"bank{bk}"], ["ftsb"], scale=sc_)
                    P.dma(FT[:, :, r0:r0 + 512].rearrange("c p t -> p c t"), ftsb, reads=["ftsb"], writes=["FT"])
                    for s in range(4):
                        ba, bb = 5 + (s % 2), 7
                        for k in range(8):
                            P.op("pe", lambda e, s=s, k=k, ba=ba: e.matmul(out=bank(ba)[:, 0:280], lhsT=hT[:, k, s * 128:(s + 1) * 128], rhs=win[:, k, 2048:2328], start=(k == 0), stop=(k == 7)),
                                 reads=["win"] + hkeys, writes=[f"bank{ba}"], pe_acc=(k > 0))
                        for k in range(8):
                            P.op("pe", lambda e, s=s, k=k: e.matmul(out=bank(7), lhsT=hT[:, k, s * 128:(s + 1) * 128], rhs=win[:, k, 2328:2840], start=(k == 0), stop=(k == 7)),
                                 reads=["win"] + hkeys, writes=["bank7"], pe_acc=(k > 0))
                        P.op("dve", lambda e, s=s, ba=ba: e.tensor_copy(out=tmv[:, s, :], in_=bank(ba)[:, 0:256]), reads=[f"bank{ba}"], writes=["tmv"])
                        P.op("act", lambda e, s=s, ba=ba: e.activation(out=gts[:, s, :], in_=bank(ba)[:, 256:280], func=AF.Sigmoid), reads=[f"bank{ba}"], writes=["gts"])
                        evac(dvs[:, s, :], bank(7), ["bank7"], ["dvs"])
                    P.dma(TMV[r0:r0 + 512, :].rearrange("(s p) c -> p s c", p=128), tmv, reads=["tmv"], writes=["TMV"])
                    P.dma(GT[r0:r0 + 512, :].rearrange("(s p) c -> p s c", p=128), gts, reads=["gts"], writes=["GT"])
                    P.dma(DV[r0:r0 + 512, :].rearrange("(s p) c -> p s c", p=128), dvs, reads=["dvs"], writes=["DV"])
            P.barrier()

        @_stage("cmp")
        def _st():
            aF.reset(); aB.reset()
            w1b = aB.get([64, 2, 32 * 256])
            w1st = aF.get([64, 32 * 256])
            for kv in range(2):
                load_cast(w1b[:, kv, :], w1_d[kv].rearrange("d l h -> d (l h)"), w1st, "w1b", "w1st")
            posf = aF.get([64, 64]); posb = aB.get([64, 64])
            load_cast(posb, posT_d.rearrange("d a l -> d (a l)"), posf, "posb", "posf", eng="dve")
            w2f = aF.get([128, 256]); w2b = aB.get([128, 2, 2, 64])
            load_cast(w2b.rearrange("p a b c -> p (a b c)"), w2_d.rearrange("p a b c -> p (a b c)"), w2f, "w2b", "w2f", eng="dve")
            hb = aF.get([128, 4])
            for kv in range(2):
                for half in range(2):
                    for l in range(32):
                        P.op("pe", lambda e, kv=kv, half=half, l=l: e.matmul(out=bank(0)[:, kv * 2 + half:kv * 2 + half + 1],
                                                                      lhsT=w1b[:, kv, l * 256 + half * 128:l * 256 + half * 128 + 128],
                                                                      rhs=posb[:, kv * 32 + l:kv * 32 + l + 1], start=(l == 0), stop=(l == 31)),
                             reads=["w1b", "posb"], writes=["bank0"], pe_acc=not (kv == 0 and half == 0 and l == 0))
            P.op("dve", lambda e: e.tensor_copy(out=hb, in_=bank(0)[:, 0:4]), reads=["bank0"], writes=["hb"])
            tok = aB.get([64, S]); hid = aB.get([128, 2, 256])
            kcs = aB.get([64, 256]); vcs = aF.get([128, 2, 64])
            for b in range(NB):
                for g in range(2):
                    for kv in range(2):
                        P.dma(tok, FT[4 + kv, g * 64:(g + 1) * 64, b * S:(b + 1) * S], reads=["FT"], writes=["tok"])
                        for half in range(2):
                            bk = 1 + half
                            for l in range(32):
                                P.op("pe", lambda e, kv=kv, half=half, l=l, bk=bk: e.matmul(out=bank(bk)[:, 0:C], lhsT=w1b[:, kv, l * 256 + half * 128:l * 256 + half * 128 + 128],
                                                                                     rhs=tok[:, l:l + 16 * (C - 1) + 1:16], start=(l == 0), stop=(l == 31)),
                                     reads=["w1b", "tok"], writes=[f"bank{bk}"], pe_acc=(l > 0))
                            P.op("act", lambda e, kv=kv, half=half, bk=bk: e.activation(out=hid[:, half, 0:C], in_=bank(bk)[:, 0:C], func=AF.Gelu_apprx_tanh,
                                                                                   bias=hb[:, kv * 2 + half:kv * 2 + half + 1]),
                                 reads=[f"bank{bk}", "hb"], writes=[f"hid{half}"])
                        if kv == 0:
                            for half in range(2):
                                P.op("pe", lambda e, half=half: e.matmul(out=bank(3)[0:64, 0:C], lhsT=w2b[:, 0, half, :], rhs=hid[:, half, 0:C], start=(half == 0), stop=(half == 1)),
                                     reads=["w2b", f"hid{half}"], writes=["bank3"], pe_acc=(half > 0))
                            P.op("pool", lambda e: e.memset(kcs, 0.0), writes=["kcs"])
                            P.op("dve", lambda e: e.tensor_copy(out=kcs[:, 0:C], in_=bank(3)[0:64, 0:C]), reads=["bank3"], writes=["kcs"])
                            P.dma(KC[b, g], kcs, reads=["kcs"], writes=["KC"])
                        else:
                            P.op("pool", lambda e: e.memset(vcs, 0.0), writes=["vcs"])
                            for ct in range(NCT):
                                cs = min(128, C - ct * 128)
                                for half in range(2):
                                    P.op("pe", lambda e, ct=ct, cs=cs, half=half: e.matmul(out=bank(4)[0:cs, ct * 64:(ct + 1) * 64], lhsT=hid[:, half, ct * 128:ct * 128 + cs],
                                                                                    rhs=w2b[:, 1, half, :], start=(half == 0), stop=(half == 1)),
                                         reads=["w2b", f"hid{half}"], writes=["bank4"], pe_acc=(half > 0 or ct > 0))
                            for ct in range(NCT):
                                cs = min(128, C - ct * 128)
                                P.op("dve", lambda e, ct=ct, cs=cs: e.tensor_copy(out=vcs[0:cs, ct, :], in_=bank(4)[0:cs, ct * 64:(ct + 1) * 64]), reads=["bank4"], writes=["vcs"])
                            P.dma(VC[b, g].rearrange("(ct p) d -> p ct d", p=128), vcs, reads=["vcs"], writes=["VC"])
            P.barrier()

        need_attn = any(s in stages for s in ("csel", "sel", "win", "diff"))
        if need_attn:
            aF.reset(); aB.reset()
            bD = aB.get([128, 12, 128]); bP = aB.get([128, 12, 128]); bW = aB.get([128, 4, 128])
            tf = aF.get([128, 12, 128]); r31 = aF.get([128, 12]); mDt = aF.get([128, 128])
            P.dma(r31, r31_d, writes=["r31"])
            P.dma(mDt, mD_d, writes=["mDt"])
            P.dma(tf, gD_d, writes=["tf"])
            P.op("dve", lambda e: e.tensor_tensor(out=tf, in0=tf, in1=r31.unsqueeze(2).to_broadcast([128, 12, 128]), op=ALU.subtract), reads=["tf", "r31"], writes=["tf"])
            P.op("dve", lambda e: e.tensor_tensor(out=bD, in0=tf, in1=mDt.unsqueeze(1).to_broadcast([128, 12, 128]), op=ALU.add), reads=["tf", "mDt"], writes=["bD"])
            P.dma(tf, gP_d, reads=["tf"], writes=["tf"])
            P.op("dve", lambda e: e.tensor_tensor(out=bP, in0=tf, in1=r31.unsqueeze(2).to_broadcast([128, 12, 128]), op=ALU.subtract), reads=["tf", "r31"], writes=["bP"])
            mWt = aF.get([128, 128])
            P.dma(mWt, mW_d, writes=["mWt"])
            P.op("dve", lambda e: e.tensor_copy(out=bW, in_=mWt.unsqueeze(1).to_broadcast([128, 4, 128])), reads=["mWt"], writes=["bW"])
            bC = aF.get([128, 8, 16]); mCt = aF.get([128, 16])
            P.dma(bC, gC_d, writes=["bC"])
            P.dma(mCt, mC_d, writes=["mCt"])
            P.op("dve", lambda e: e.tensor_tensor(out=bC, in0=bC, in1=r31[:, 0:8].unsqueeze(2).to_broadcast([128, 8, 16]), op=ALU.subtract), reads=["bC", "r31"], writes=["bC"])
            P.op("dve", lambda e: e.tensor_tensor(out=bC, in0=bC, in1=mCt.unsqueeze(1).to_broadcast([128, 8, 16]), op=ALU.add), reads=["bC", "mCt"], writes=["bC"])
            attnF0, attnB0 = aF.off, aB.off

        @_stage("csel")
        def _st():
            aF.off, aB.off = attnF0, attnB0
            LO = aF.get([128, NQT, 64]); HI = aF.get([128, NQT, 64]); C2S = aF.get([128, 2, 64])
            P.dma(LO, LO_d, writes=["LO"]); P.dma(HI, HI_d, writes=["HI"]); P.dma(C2S, C2S_d, writes=["C2S"])
            kc = aB.get([64, 256]); vc = aF.get([128, 2, 64])
            qh = aB.get([64, 4, 128])
            pc = aF.get([128, 4, 256]); pT = aF.get([128, 2, 512])
            mx = aF.get([128, 4]); Z = aF.get([128, 4])
            scs = aF.get([128, 64]); rep = aF.get([128, 64]); m8 = aF.get([128, 16]); nm = aF.get([128, 64])
            ocs = aF.get([128, 256]); nmT = aB.get([64, S])
            pS = psum_t[:, 0:1024].rearrange("p (h c) -> p h c", c=256)
            for b in range(NB):
                for g in range(2):
                    P.dma(kc, KC[b, g], reads=["KC"], writes=["kc"])
                    P.dma(vc, VC[b, g].rearrange("(ct p) d -> p ct d", p=128), reads=["VC"], writes=["vc"])
                    for qt in range(NQT):
                        t0 = b * S + qt * 128
                        ncols = min(8 * qt + 7, C)
                        nct = (ncols + 127) // 128
                        P.dma(qh, FT[2 * g:2 * g + 2, :, t0:t0 + 128].rearrange("c (two d) t -> d (c two) t", two=2), reads=["FT"], writes=["qh"])
                        for hh in range(4):
                            P.op("pe", lambda e, hh=hh, ncols=ncols: e.matmul(out=pS[:, hh, 0:ncols], lhsT=qh[:, hh, :], rhs=kc[:, 0:ncols], start=True, stop=True),
                                 reads=["qh", "kc"], writes=[f"bank{hh // 2}"])
                        cw0 = 8 * qt - 9
                        w0 = max(cw0, 0)
                        P.op("dve", lambda e, w0=w0, cw0=cw0, ncols=ncols, g=g: e.tensor_tensor(out=pS[:, :, w0:ncols], in0=pS[:, :, w0:ncols],
                                                                                         in1=bC[:, 4 * g:4 * g + 4, w0 - cw0:ncols - cw0], op=ALU.add),
                             reads=["bank0", "bank1", "bC"], writes=["bank0", "bank1"])
                        P.op("dve", lambda e, ncols=ncols: e.tensor_reduce(out=mx, in_=pS[:, :, 0:ncols], axis=AX.X, op=ALU.max), reads=["bank0", "bank1"], writes=["mx"])
                        P.op("dve", lambda e: e.tensor_scalar(out=mx, in0=mx, scalar1=-1000.0, scalar2=-1.0, op0=ALU.max, op1=ALU.mult), reads=["mx"], writes=["mx"])
                        for hh in range(4):
                            P.op("act", lambda e, hh=hh, ncols=ncols: e.activation(out=pc[:, hh, 0:ncols], in_=pS[:, hh, 0:ncols], func=AF.Exp, bias=mx[:, hh:hh + 1], accum_out=Z[:, hh:hh + 1]),
                                 reads=[f"bank{hh // 2}", "mx"], writes=["pc", "Z"])
                        P.op("dve", lambda e: e.tensor_scalar(out=Z, in0=Z, scalar1=1e-30, scalar2=None, op0=ALU.add), reads=["Z"], writes=["Z"])
                        P.op("dve", lambda e: e.reciprocal(out=Z, in_=Z), reads=["Z"], writes=["Z"])
                        P.op("dve", lambda e, ncols=ncols: e.tensor_tensor(out=pc[:, :, 0:ncols], in0=pc[:, :, 0:ncols], in1=Z.unsqueeze(2).to_broadcast([128, 4, ncols]), op=ALU.mult),
                             reads=["pc", "Z"], writes=["pc"])
                        for ct in range(nct):
                            cs = min(128, ncols - ct * 128)
                            for hh in range(4):
                                P.op("pe", lambda e, ct=ct, cs=cs, hh=hh: e.transpose(out=bank(2 + ct)[0:cs, hh * 128:(hh + 1) * 128], in_=pc[:, hh, ct * 128:ct * 128 + cs], identity=ident),
                                     reads=["pc", "ident"], writes=[f"bank{2 + ct}"], pe_acc=(hh > 0))
                            evac(pT[0:cs, ct, :], bank(2 + ct)[0:cs, :], [f"bank{2 + ct}"], ["pT"])
                        first = True
                        for ct in range(nct):
                            cs = min(128, ncols - ct * 128)
                            for hh in range(4):
                                last = (ct == nct - 1 and hh == 3)
                                P.op("pe", lambda e, ct=ct, cs=cs, hh=hh, first=first, last=last: e.matmul(out=bank(4)[:, 0:64], lhsT=pT[0:cs, ct, hh * 128:(hh + 1) * 128], rhs=C2S[0:cs, ct, :], start=first, stop=last),
                                     reads=["pT", "C2S"], writes=["bank4"], pe_acc=not first)
                                first = False
                        for hh in range(4):
                            for ct in range(nct):
                                cs = min(128, ncols - ct * 128)
                                P.op("pe", lambda e, ct=ct, cs=cs, hh=hh, nct=nct: e.matmul(out=bank(5)[:, hh * 64:(hh + 1) * 64], lhsT=pT[0:cs, ct, hh * 128:(hh + 1) * 128], rhs=vc[0:cs, ct, :], start=(ct == 0), stop=(ct == nct - 1)),
                                     reads=["pT", "vc"], writes=["bank5"], pe_acc=not (hh == 0 and ct == 0))
                        P.op("act", lambda e: e.activation(out=ocs, in_=bank(5)[:, 0:256], func=AF.Copy), reads=["bank5"], writes=["ocs"])
                        P.dma(OC[t0:t0 + 128, g * 256:(g + 1) * 256], ocs, reads=["ocs"], writes=["OC"])
                        P.op("dve", lambda e, qt=qt: e.tensor_tensor(out=scs, in0=bank(4)[:, 0:64], in1=LO[:, qt, :], op=ALU.max), reads=["bank4", "LO"], writes=["scs"])
                        P.op("dve", lambda e, qt=qt: e.tensor_tensor(out=scs, in0=scs, in1=HI[:, qt, :], op=ALU.min), reads=["scs", "HI"], writes=["scs"])
                        P.op("dve", lambda e: e.max(out=m8[:, 0:8], in_=scs), reads=["scs"], writes=["m8"])
                        P.op("dve", lambda e: e.match_replace(out=rep, in_to_replace=m8[:, 0:8], in_values=scs, imm_value=-3e38), reads=["scs", "m8"], writes=["rep"])
                        P.op("dve", lambda e: e.max(out=m8[:, 8:16], in_=rep), reads=["rep", "m8"], writes=["m8"])
                        P.op("dve", lambda e: e.tensor_scalar(out=nm, in0=scs, scalar1=m8[:, 15:16], scalar2=NEG, op0=ALU.is_lt, op1=ALU.mult), reads=["scs", "m8"], writes=["nm"])
                        P.op("pe", lambda e: e.transpose(out=bank(6)[0:64, 0:128], in_=nm, identity=ident), reads=["nm", "ident"], writes=["bank6"])
                        P.op("act", lambda e, qt=qt: e.activation(out=nmT[:, qt * 128:(qt + 1) * 128], in_=bank(6)[0:64, 0:128], func=AF.Copy), reads=["bank6"], writes=["nmT"])
                    P.dma(NM[b, g], nmT, reads=["nmT"], writes=["NM"])
            P.barrier()

        def nsa_branch(branch):
            aF.off, aB.off = attnF0, attnB0
            is_sel = (branch == 1)
            kT = aB.get([64, S]); vs = aB.get([128, NQT, 65]); qhs = [aB.get([64, 512]) for _ in range(3)]
            pTs = [aB.get([128, 512]) for _ in range(3)]
            nmS = aB.get([64, S]) if is_sel else None
            nmreps = [aB.get([64, 4, 128]) for _ in range(3)] if is_sel else None
            W2 = None
            if is_sel:
                W2 = aB.get([64, 4096]); w2f = aF.get([64, 4096])
                load_cast(W2, W2_d, w2f, "W2", "w2f", eng="dve")
            rzs = [aF.get([128, 4]) for _ in range(2)]; osbs = [aF.get([128, 4, 64]) for _ in range(2)]
            ODST = OS if is_sel else OW
            P.op("pool", lambda e: e.memset(vs[:, :, 64:65], 1.0), writes=["vs1"])
            LA = 2
            for b in range(NB):
                for g in range(2):
                    P.dma(kT, FT[5 + branch, g * 64:(g + 1) * 64, b * S:(b + 1) * S], reads=["FT"], writes=["kT"])
                    P.dma(vs[:, :, 0:64], TMV[b * S:(b + 1) * S, (branch - 1) * 128 + g * 64:(branch - 1) * 128 + (g + 1) * 64].rearrange("(kt p) d -> p kt d", p=128),
                          reads=["TMV"], writes=["vs"])
                    if is_sel:
                        P.dma(nmS, NM[b, g], reads=["NM"], writes=["nmS"])
                    jobs = []
                    for qt in range(NQT):
                        kts = list(range(0, qt + 1)) if is_sel else list(range(max(0, qt - 4), qt + 1))
                        for ii, kt in enumerate(kts):
                            jobs.append((qt, ii, kt, len(kts)))

                    def prologue(qt, b=b, g=g):
                        t0 = b * S + qt * 128
                        q3 = qt % 3
                        P.dma(qhs[q3].rearrange("d (h t) -> d h t", h=4), FT[2 * g:2 * g + 2, :, t0:t0 + 128].rearrange("c (two d) t -> d (c two) t", two=2), reads=["FT"], writes=[f"qh{q3}"])
                        if is_sel:
                            P.op("pool", lambda e, qt=qt, q3=q3: e.tensor_copy(out=nmreps[q3], in_=nmS[:, qt * 128:(qt + 1) * 128].unsqueeze(1).to_broadcast([64, 4, 128])),
                                 reads=["nmS"], writes=[f"nmrep{q3}"])

                    def qk_part(j, g=g):
                        qt, ii, kt, n = jobs[j]
                        bk = j % 3
                        q3 = qt % 3
                        mm = [(kT[:, kt * 128:(kt + 1) * 128], qhs[q3], ["kT", f"qh{q3}"])]
                        if kt == qt:
                            mm.append((identb, bD[:, 4 * g:4 * g + 4, :].rearrange("p h t -> p (h t)"), ["identb", "bD"]))
                        elif kt == qt - 1:
                            mm.append((identb, bP[:, 4 * g:4 * g + 4, :].rearrange("p h t -> p (h t)"), ["identb", "bP"]))
                        elif (not is_sel) and kt == qt - 4:
                            mm.append((identb, bW.rearrange("p h t -> p (h t)"), ["identb", "bW"]))
                        if is_sel:
                            mm.append((W2[:, kt * 128:(kt + 1) * 128], nmreps[q3].rearrange("p h t -> p (h t)"), ["W2", f"nmrep{q3}"]))
                        for mi, (l_, r_, rd) in enumerate(mm):
                            P.op("pe", lambda e, l_=l_, r_=r_, mi=mi, n_=len(mm), bk=bk: e.matmul(out=bank(bk), lhsT=l_, rhs=r_, start=(mi == 0), stop=(mi == n_ - 1)),
                                 reads=rd, writes=[f"bank{bk}"], pe_acc=(mi > 0))

                    def pv_part(j, b=b, g=g):
                        qt, ii, kt, n = jobs[j]
                        bk = j % 3
                        pk = f"pT{bk}"
                        P.op("act", lambda e, bk=bk: e.activation(out=pTs[bk], in_=bank(bk), func=AF.Exp), reads=[f"bank{bk}"], writes=[pk])
                        for hh in range(4):
                            P.op("pe", lambda e, hh=hh, bk=bk, kt=kt, ii=ii, n=n: e.matmul(out=bank(3 + hh)[:, 0:65], lhsT=pTs[bk][:, hh * 128:(hh + 1) * 128], rhs=vs[:, kt, :],
                                                                                     start=(ii == 0), stop=(ii == n - 1)),
                                 reads=[pk, "vs", "vs1"], writes=[f"bank{3 + hh}"], pe_acc=(ii > 0))
                        if ii == n - 1:
                            t0 = b * S + qt * 128
                            qp = qt % 2
                            rz = rzs[qp]; osb = osbs[qp]
                            for hh in range(4):
                                P.op("dve", lambda e, hh=hh, rz=rz: e.reciprocal(out=rz[:, hh:hh + 1], in_=bank(3 + hh)[:, 64:65]), reads=[f"bank{3 + hh}"], writes=[f"rz{qp}_{hh}"])
                                P.op("dve", lambda e, hh=hh, rz=rz, osb=osb: e.tensor_scalar(out=osb[:, hh, :], in0=bank(3 + hh)[:, 0:64], scalar1=rz[:, hh:hh + 1], scalar2=None, op0=ALU.mult),
                                     reads=[f"bank{3 + hh}", f"rz{qp}_{hh}"], writes=[f"osb{qp}"])
                            P.dma(ODST[t0:t0 + 128, g * 256:(g + 1) * 256], osb.rearrange("p h d -> p (h d)"), reads=[f"osb{qp}"], writes=["ODST"])

                    for i in range(len(jobs) + LA):
                        if i < len(jobs):
                            if jobs[i][1] == 0:
                                prologue(jobs[i][0])
                            qk_part(i)
                        if i >= LA:
                            pv_part(i - LA)
            P.barrier()

        if "sel" in stages:
            nsa_branch(1)
        if "win" in stages:
            nsa_branch(2)

        @_stage("diff")
        def _st():
            aF.off, aB.off = attnF0, attnB0
            dk = aB.get([64, 2, S]); dvv = aB.get([128, NQT, 129]); dqs = [aB.get([64, 2, 128]) for _ in range(3)]
            pTs = [aB.get([128, 128]) for _ in range(4)]
            subg = aF.get([128, 128]); rz = aF.get([128, 2]); o1 = aF.get([128, 128]); o2 = aF.get([128, 128])
            sq = aF.get([128, 128]); ss = aF.get([128, 1]); odss = [aF.get([128, 128]) for _ in range(2)]
            P.dma(subg, subg_d.partition_broadcast(128), writes=["subg"])
            P.op("dve", lambda e: e.tensor_scalar(out=subg, in0=subg, scalar1=0.8, scalar2=None, op0=ALU.mult), reads=["subg"], writes=["subg"])
            P.op("pool", lambda e: e.memset(dvv[:, :, 128:129], 1.0), writes=["dv1"])
            LA = 2
            for b in range(NB):
                for h in range(4):
                    P.dma(dk, FT[12 + h, :, b * S:(b + 1) * S].rearrange("(m d) t -> d m t", m=2), reads=["FT"], writes=["dk"])
                    P.dma(dvv[:, :, 0:128], DV[b * S:(b + 1) * S, h * 128:(h + 1) * 128].rearrange("(kt p) d -> p kt d", p=128), reads=["DV"], writes=["dvv"])
                    jobs = []
                    for qt in range(NQT):
                        for m in range(2):
                            for kt in range(qt + 1):
                                jobs.append((qt, m, kt))

                    def prologue(qt, b=b, h=h):
                        t0 = b * S + qt * 128
                        q3 = qt % 3
                        P.dma(dqs[q3], FT[8 + h, :, t0:t0 + 128].rearrange("(m d) t -> d m t", m=2), reads=["FT"], writes=[f"dq{q3}"])

                    def qk_part(j, h=h):
                        qt, m, kt = jobs[j]
                        bk = j % 4
                        q3 = qt % 3
                        mm = [(dk[:, m, kt * 128:(kt + 1) * 128], dqs[q3][:, m, :], ["dk", f"dq{q3}"])]
                        if kt == qt:
                            mm.append((identb, bD[:, 8 + h, :], ["identb", "bD"]))
                        elif kt == qt - 1:
                            mm.append((identb, bP[:, 8 + h, :], ["identb", "bP"]))
                        for mi, (l_, r_, rd) in enumerate(mm):
                            P.op("pe", lambda e, l_=l_, r_=r_, mi=mi, n_=len(mm), bk=bk: e.matmul(out=bank(bk)[:, 0:128], lhsT=l_, rhs=r_, start=(mi == 0), stop=(mi == n_ - 1)),
                                 reads=rd, writes=[f"bank{bk}"], pe_acc=(mi > 0))

                    def pv_part(j, b=b, h=h):
                        qt, m, kt = jobs[j]
                        bk = j % 4
                        P.op("act", lambda e, bk=bk: e.activation(out=pTs[bk], in_=bank(bk)[:, 0:128], func=AF.Exp), reads=[f"bank{bk}"], writes=[f"dpT{bk}"])
                        P.op("pe", lambda e, bk=bk, m=m, kt=kt, qt=qt: e.matmul(out=bank(4 + m)[:, 0:129], lhsT=pTs[bk], rhs=dvv[:, kt, :], start=(kt == 0), stop=(kt == qt)),
                             reads=[f"dpT{bk}", "dvv", "dv1"], writes=[f"bank{4 + m}"], pe_acc=(kt > 0))
                        if m == 1 and kt == qt:
                            t0 = b * S + qt * 128
                            qp = qt % 2
                            ods = odss[qp]
                            P.op("dve", lambda e: e.reciprocal(out=rz[:, 0:1], in_=bank(4)[:, 128:129]), reads=["bank4"], writes=["drz"])
                            P.op("dve", lambda e: e.reciprocal(out=rz[:, 1:2], in_=bank(5)[:, 128:129]), reads=["bank5", "drz"], writes=["drz"])
                            P.op("dve", lambda e: e.tensor_scalar(out=o1, in0=bank(4)[:, 0:128], scalar1=rz[:, 0:1], scalar2=None, op0=ALU.mult), reads=["bank4", "drz"], writes=["o1"])
                            P.op("dve", lambda e: e.tensor_scalar(out=o2, in0=bank(5)[:, 0:128], scalar1=rz[:, 1:2], scalar2=None, op0=ALU.mult), reads=["bank5", "drz"], writes=["o2"])
                            P.op("dve", lambda e: e.scalar_tensor_tensor(out=o1, in0=o2, scalar=neglam[:, 0:1], in1=o1, op0=ALU.mult, op1=ALU.add), reads=["o1", "o2"], writes=["o1"])
                            P.op("act", lambda e: e.activation(out=sq, in_=o1, func=AF.Square, accum_out=ss), reads=["o1"], writes=["dsq", "dss"])
                            P.op("act", lambda e: e.activation(out=ss, in_=ss, func=AF.Sqrt, scale=1.0 / 128, bias=epsb), reads=["dss", "epsb"], writes=["dss"])
                            P.op("dve", lambda e: e.reciprocal(out=ss, in_=ss), reads=["dss"], writes=["dss"])
                            P.op("dve", lambda e, ods=ods: e.scalar_tensor_tensor(out=ods, in0=o1, scalar=ss[:, 0:1], in1=subg, op0=ALU.mult, op1=ALU.mult), reads=["o1", "dss", "subg"], writes=[f"ods{qp}"])
                            P.dma(OD[t0:t0 + 128, h * 128:(h + 1) * 128], ods, reads=[f"ods{qp}"], writes=["OD"])

                    for i in range(len(jobs) + LA):
                        if i < len(jobs):
                            if jobs[i][1] == 0 and jobs[i][2] == 0:
                                prologue(jobs[i][0])
                            qk_part(i)
                        if i >= LA:
                            pv_part(i - LA)
            P.barrier()

        @_stage("comb")
        def _st():
            aF.reset(); aB.reset()
            wout = aB.get([128, 8, 1024]); wst = aF.get([128, 1024])
            for k in range(8):
                load_cast(wout[:, k, :], wout_d[k], wst, "wout", "wst", eng=("pool", "dve")[k % 2])
            gaB = aF.get([128, 1024])
            oc = aF.get([128, 8, 64]); osx = aF.get([128, 8, 64]); ow = aF.get([128, 8, 64]); gt = aF.get([128, 8, 3])
            mix = aF.get([128, 1024]); mixT = aB.get([128, 8, 128]); xt = aF.get([128, 1024]); x1 = aF.get([128, 1024])
            for b in range(NB):
                P.dma(gaB, MODB[b, 0], reads=["MODB"], writes=["gaB"])
                for qt in range(NQT):
                    t0 = b * S + qt * 128
                    P.dma(oc.rearrange("p h d -> p (h d)"), OC[t0:t0 + 128, :], reads=["OC"], writes=["oc"])
                    P.dma(osx.rearrange("p h d -> p (h d)"), OS[t0:t0 + 128, :], reads=["ODST"], writes=["osx"])
                    P.dma(ow.rearrange("p h d -> p (h d)"), OW[t0:t0 + 128, :], reads=["ODST"], writes=["ow"])
                    P.dma(gt.rearrange("p h c -> p (h c)"), GT[t0:t0 + 128, :], reads=["GT"], writes=["gt"])
                    P.dma(mix[:, 512:1024], OD[t0:t0 + 128, :], reads=["OD"], writes=["mixd"])
                    P.dma(xt, x_d[t0:t0 + 128, :], writes=["cxt"])
                    mv = mix[:, 0:512].rearrange("p (h d) -> p h d", d=64)
                    for (src, gi, key) in ((oc, 0, "oc"), (osx, 1, "osx"), (ow, 2, "ow")):
                        P.op("dve", lambda e, src=src, gi=gi: e.tensor_tensor(out=src, in0=src, in1=gt[:, :, gi:gi + 1].to_broadcast([128, 8, 64]), op=ALU.mult),
                             reads=[key, "gt"], writes=[key])
                    P.op("pool", lambda e: e.tensor_tensor(out=mv, in0=oc, in1=osx, op=ALU.add), reads=["oc", "osx"], writes=["mixn"])
                    P.op("pool", lambda e: e.tensor_tensor(out=mv, in0=mv, in1=ow, op=ALU.add), reads=["mixn", "ow"], writes=["mixn"])
                    for k in range(8):
                        bk = k // 4
                        P.op("pe", lambda e, k=k, bk=bk: e.transpose(out=bank(bk)[:, (k % 4) * 128:(k % 4 + 1) * 128], in_=mix[:, k * 128:(k + 1) * 128], identity=ident),
                             reads=["mixn", "mixd", "ident"], writes=[f"bank{bk}"], pe_acc=(k % 4 > 0))
                    for bk in range(2):
                        evac(mixT[:, bk * 4:(bk + 1) * 4, :].rearrange("p k t -> p (k t)"), bank(bk), [f"bank{bk}"], ["mixT"])
                    for nb2 in range(2):
                        for k in range(8):
                            P.op("pe", lambda e, k=k, nb2=nb2: e.matmul(out=bank(2 + nb2), lhsT=mixT[:, k, :], rhs=wout[:, k, nb2 * 512:(nb2 + 1) * 512], start=(k == 0), stop=(k == 7)),
                                 reads=["mixT", "wout"], writes=[f"bank{2 + nb2}"], pe_acc=(k > 0))
                        P.op("dve", lambda e, nb2=nb2: e.tensor_tensor(out=x1[:, nb2 * 512:(nb2 + 1) * 512], in0=bank(2 + nb2), in1=gaB[:, nb2 * 512:(nb2 + 1) * 512], op=ALU.mult),
                             reads=[f"bank{2 + nb2}", "gaB"], writes=[f"x1_{nb2}"])
                        P.op("pool", lambda e, nb2=nb2: e.tensor_tensor(out=x1[:, nb2 * 512:(nb2 + 1) * 512], in0=x1[:, nb2 * 512:(nb2 + 1) * 512], in1=xt[:, nb2 * 512:(nb2 + 1) * 512], op=ALU.add),
                             reads=[f"x1_{nb2}", "cxt"], writes=[f"x1_{nb2}"])
                    P.dma(X1[t0:t0 + 128, :], x1, reads=["x1_0", "x1_1"], writes=["X1"])
            P.barrier()

        @_stage("peer")
        def _st():
            set_split(15 * 1024)
            NSUB = TP // 128
            wq = aB.get([128, 8, 2048])
            wst = [aF.get([128, 2048]) for _ in range(2)]
            for k in range(8):
                load_cast(wq[:, k, :], wq_d[k], wst[k % 2], "wq", f"wst{k % 2}", eng=("pool", "dve")[k % 2])
            skT = aB.get([128, 2, 128]); skf = aF.get([128, 256])
            load_cast(skT.rearrange("p a b -> p (a b)"), skT_d.rearrange("p a b -> p (a b)"), skf, "skT", "skf", eng="dve")
            P.barrier()
            aF.reset()
            gfB = aF.get([128, 1024]); fingB = aF.get([128, 1024])
            P.dma(fingB, fing_d.partition_broadcast(128), writes=["fingB"])
            x1s = aF.get([128, NSUB, 1024]); xn = aF.get([128, 1024]); scr = aF.get([128, 1024])
            ssq = aF.get([128, NSUB]); rstd = aF.get([128, NSUB])
            scb = aF.get([128, 16, 128]); sv = aF.get([128, 16, 16]); rep = aF.get([128, 128])
            cand = aF.get([128, 8, 256]); rep2 = aF.get([128, 256]); b16 = aF.get([128, 8, 24])
            tau = aF.get([128, 8]); e16 = aF.get([128, 8, 16]); Zp = aF.get([128, 8])
            tmj = aF.get([128, 4, 128])
            trT = aF.get([128, NSUB, 4, 128])
            h2T = aB.get([128, 8, TP]); qT = aB.get([128, 16, TP])
            ActT = aB.get([128, 128, TP])
            GI = 2
            NUB = 4
            ubs = [aB.get([128, GI, 1024]) for _ in range(NUB)]
            qrep = [aB.get([128, 2, 128]) for _ in range(4)]
            Eb = [aF.get([128, 128]) for _ in range(4)]
            Ab = [aB.get([128, 128]) for _ in range(4)]
            Bb = [aB.get([128, 128]) for _ in range(4)]
            ysb = aF.get([128, 1024])
            for b in range(NB):
                P.dma(gfB, MODB[b, 1], reads=["MODB"], writes=["gfB"])
                for tp in range(S // TP):
                    T0 = b * S + tp * TP
                    P.mark(f"A{b}_{tp}")
                    P.dma(x1s, X1[T0:T0 + TP, :].rearrange("(s p) d -> p s d", p=128), reads=["X1"], writes=["x1s"])
                    rms_rows(x1s, NSUB, ssq, rstd, scr, "x1s", "p2")
                    for s in range(NSUB):
                        P.op("dve", lambda e, s=s: e.tensor_scalar(out=xn, in0=x1s[:, s, :], scalar1=rstd[:, s:s + 1], scalar2=None, op0=ALU.mult),
                             reads=["x1s", "p2rstd"], writes=["pxn"])
                        for k in range(8):
                            bk = k // 4
                            P.op("pe", lambda e, k=k, bk=bk: e.transpose(out=bank(bk)[:, (k % 4) * 128:(k % 4 + 1) * 128], in_=xn[:, k * 128:(k + 1) * 128], identity=ident),
                                 reads=["pxn", "ident"], writes=[f"bank{bk}"], pe_acc=(k % 4 > 0))
                        for k in range(8):
                            bk = k // 4
                            P.op("act", lambda e, k=k, bk=bk, s=s, b=b: e.activation(out=h2T[:, k, s * 128:(s + 1) * 128], in_=bank(bk)[:, (k % 4) * 128:(k % 4 + 1) * 128], func=AF.Identity,
                                                                             scale=A2T[:, k, b:b + 1], bias=B2T[:, k, b:b + 1]),
                                 reads=[f"bank{bk}"], writes=["h2T"])
                    P.mark(f"B{b}_{tp}")
                    for hp in range(16):
                        bk = 2 + hp % 2
                        for k in range(8):
                            P.op("pe", lambda e, hp=hp, k=k, bk=bk: e.matmul(out=bank(bk)[:, 0:TP], lhsT=wq[:, k, hp * 128:(hp + 1) * 128], rhs=h2T[:, k, :], start=(k == 0), stop=(k == 7)),
                                 reads=["wq", "h2T"], writes=[f"bank{bk}"], pe_acc=(k > 0))
                        evac(qT[:, hp, :], bank(bk)[:, 0:TP], [f"bank{bk}"], ["qT"])
                    P.mark(f"C{b}_{tp}")
                    c_rec = []
                    real_op = P.op
                    P.op = lambda *a, **k: c_rec.append((a, k))
                    for s in range(NSUB):
                        for hp in range(16):
                            bk = 4 + hp // 4
                            P.op("pe", lambda e, hp=hp, bk=bk, s=s: e.matmul(out=bank(bk)[:, (hp % 4) * 128:(hp % 4 + 1) * 128], lhsT=qT[:, hp, s * 128:(s + 1) * 128], rhs=skT[:, hp % 2, :], start=True, stop=True),
                                 reads=["qT", "skT"], writes=[f"bank{bk}"], pe_acc=(hp % 4 > 0))
                        for q4 in range(4):
                            evac(scb[:, q4 * 4:(q4 + 1) * 4, :].rearrange("p a b -> p (a b)"), bank(4 + q4), [f"bank{4 + q4}"], ["scb"])
                        for hp in range(16):
                            P.op("dve", lambda e, hp=hp: e.max(out=sv[:, hp, 0:8], in_=scb[:, hp, :]), reads=["scb"], writes=["sv"])
                            P.op("dve", lambda e, hp=hp: e.match_replace(out=rep, in_to_replace=sv[:, hp, 0:8], in_values=scb[:, hp, :], imm_value=-1e30), reads=["scb", "sv"], writes=["rep"])
                            P.op("dve", lambda e, hp=hp: e.max(out=sv[:, hp, 8:16], in_=rep), reads=["rep", "sv"], writes=["sv"])
                        sv4 = sv.rearrange("p (h two) k -> p h two k", two=2)
                        P.op("dve", lambda e, sv4=sv4: e.tensor_tensor(out=cand.rearrange("p h (a c) -> p h a c", c=16), in0=sv4[:, :, 0, :].unsqueeze(3).to_broadcast([128, 8, 16, 16]),
                                                                in1=sv4[:, :, 1, :].unsqueeze(2).to_broadcast([128, 8, 16, 16]), op=ALU.add), reads=["sv"], writes=["cand"])
                        for h in range(8):
                            P.op("dve", lambda e, h=h: e.max(out=b16[:, h, 0:8], in_=cand[:, h, :]), reads=["cand"], writes=["b16"])
                            P.op("dve", lambda e, h=h: e.match_replace(out=rep2, in_to_replace=b16[:, h, 0:8], in_values=cand[:, h, :], imm_value=-1e30), reads=["cand", "b16"], writes=["rep2"])
                            P.op("dve", lambda e, h=h: e.max(out=b16[:, h, 8:16], in_=rep2), reads=["rep2", "b16"], writes=["b16"])
                            P.op("dve", lambda e, h=h: e.match_replace(out=rep2, in_to_replace=b16[:, h, 8:16], in_values=rep2, imm_value=-1e30), reads=["rep2", "b16"], writes=["rep2"])
                            P.op("dve", lambda e, h=h: e.max(out=b16[:, h, 16:24], in_=rep2), reads=["rep2", "b16"], writes=["b16"])
                        P.op("dve", lambda e: e.tensor_tensor(out=tau, in0=b16[:, :, 15], in1=b16[:, :, 16], op=ALU.add), reads=["b16"], writes=["tau"])
                        P.op("dve", lambda e: e.tensor_scalar(out=tau, in0=tau, scalar1=0.5, scalar2=None, op0=ALU.mult), reads=["tau"], writes=["tau"])
                        P.op("dve", lambda e: e.tensor_tensor(out=e16, in0=b16[:, :, 0:16], in1=b16[:, :, 0:1].to_broadcast([128, 8, 16]), op=ALU.subtract), reads=["b16"], writes=["e16"])
                        P.op("act", lambda e: e.activation(out=e16, in_=e16, func=AF.Exp), reads=["e16"], writes=["e16"])
                        P.op("dve", lambda e: e.tensor_reduce(out=Zp, in_=e16, axis=AX.X, op=ALU.add), reads=["e16"], writes=["Zp"])
                        P.op("dve", lambda e: e.reciprocal(out=Zp, in_=Zp), reads=["Zp"], writes=["Zp"])
                        tm4 = tmj.rearrange("p w (h c) -> p w h c", c=16)
                        sv0 = sv4[:, :, 0, :]; sv1 = sv4[:, :, 1, :]
                        P.op("dve", lambda e, sv1=sv1, tm4=tm4: e.tensor_tensor(out=tm4[:, 0], in0=tau.unsqueeze(2).to_broadcast([128, 8, 16]), in1=sv1, op=ALU.subtract), reads=["tau", "sv"], writes=["tmj"])
                        P.op("dve", lambda e, sv1=sv1, tm4=tm4: e.tensor_tensor(out=tm4[:, 1], in0=sv1, in1=sv1[:, :, 0:1].to_broadcast([128, 8, 16]), op=ALU.subtract), reads=["sv", "tmj"], writes=["tmj"])
                        P.op("act", lambda e, tm4=tm4: e.activation(out=tm4[:, 1], in_=tm4[:, 1], func=AF.Exp), reads=["tmj"], writes=["tmj"])
                        P.op("dve", lambda e, tm4=tm4: e.tensor_tensor(out=tm4[:, 1], in0=tm4[:, 1], in1=Zp.unsqueeze(2).to_broadcast([128, 8, 16]), op=ALU.mult), reads=["tmj", "Zp"], writes=["tmj"])
                        P.op("dve", lambda e, sv0=sv0, tm4=tm4: e.tensor_scalar(out=tm4[:, 2], in0=sv0[:, :, 0:1].to_broadcast([128, 8, 16]), scalar1=-1.0, scalar2=None, op0=ALU.mult), reads=["sv", "tmj"], writes=["tmj"])
                        P.op("dve", lambda e, sv1=sv1, tm4=tm4: e.tensor_copy(out=tm4[:, 3], in_=sv1), reads=["sv", "tmj"], writes=["tmj"])
                        for w in range(4):
                            P.op("pe", lambda e, w=w: e.transpose(out=bank(0)[:, w * 128:(w + 1) * 128], in_=tmj[:, w, :], identity=ident), reads=["tmj", "ident"], writes=["bank0"], pe_acc=(w > 0))
                        P.op("act", lambda e, s=s: e.activation(out=trT[:, s, :, :].rearrange("p w t -> p (w t)"), in_=bank(0), func=AF.Copy), reads=["bank0"], writes=["trT"])
                    P.op = real_op
                    P.mark(f"D{b}_{tp}")
                    for ig in range(128 // GI):
                        r = ig % NUB
                        P.dma(ubs[r], UB[ig * GI:(ig + 1) * GI].rearrange("i p k e -> p i (k e)"), reads=["UBVB"], writes=[f"ubs{r}"])
                        for ii in range(GI):
                            i = ig * GI + ii
                            bk = 1 + i % 2
                            for k in range(8):
                                P.op("pe", lambda e, r=r, ii=ii, k=k, bk=bk: e.matmul(out=bank(bk)[:, 0:TP], lhsT=ubs[r][:, ii, k * 128:(k + 1) * 128], rhs=h2T[:, k, :], start=(k == 0), stop=(k == 7)),
                                     reads=[f"ubs{r}", "h2T"], writes=[f"bank{bk}"], pe_acc=(k > 0))
                            P.op("act", lambda e, i=i, bk=bk: e.activation(out=ActT[:, i, :], in_=bank(bk)[:, 0:TP], func=AF.Gelu_apprx_tanh), reads=[f"bank{bk}"], writes=["ActT"])
                        n_ig = 128 // GI
                        c_lo = (len(c_rec) * ig) // n_ig
                        c_hi = (len(c_rec) * (ig + 1)) // n_ig
                        for (a_, k_) in c_rec[c_lo:c_hi]:
                            P.op(*a_, **k_)
                    P.mark(f"E{b}_{tp}")
                    def e_front(t):
                        r4 = t % 4
                        qv = qT[:, :, t].rearrange("p (h two) -> p h two", two=2)
                        q_in = qv.rearrange("p h two -> p two h").unsqueeze(3).to_broadcast([128, 2, 8, 16])
                        q_out = qrep[r4].rearrange("p two (h c) -> p two h c", c=16)
                        if t % 2 == 0:
                            P.op("pool", lambda e, q_in=q_in, q_out=q_out: e.tensor_copy(out=q_out, in_=q_in), reads=["qT"], writes=[f"qrep{r4}"])
                        else:
                            P.op("act", lambda e, q_in=q_in, q_out=q_out: e.activation(out=q_out, in_=q_in, func=AF.Copy), reads=["qT"], writes=[f"qrep{r4}"])
                        pB = bank(r4)
                        for p_ in range(2):
                            P.op("pe", lambda e, r4=r4, p_=p_, pB=pB: e.matmul(out=pB[:, p_ * 128:(p_ + 1) * 128], lhsT=qrep[r4][:, p_, :], rhs=skT[:, p_, :], start=True, stop=True),
                                 reads=[f"qrep{r4}", "skT"], writes=[f"bank{r4}"], pe_acc=(p_ > 0))

                    def e_mid(t):
                        s, tl = t // 128, t % 128
                        r2 = t % 4
                        pB = bank(r2)
                        P.op("act", lambda e, r2=r2, pB=pB, s=s, tl=tl: e.activation(out=Eb[r2], in_=pB[:, 0:128], func=AF.Exp, bias=trT[:, s, 2, tl:tl + 1]),
                             reads=[f"bank{r2}", "trT"], writes=[f"Eb{r2}"])
                        P.op("dve", lambda e, r2=r2, pB=pB, s=s, tl=tl: e.scalar_tensor_tensor(out=Ab[r2], in0=pB[:, 0:128], scalar=trT[:, s, 0, tl:tl + 1], in1=Eb[r2], op0=ALU.is_ge, op1=ALU.mult),
                             reads=[f"bank{r2}", "trT", f"Eb{r2}"], writes=[f"Ab{r2}"])
                        P.op("dve", lambda e, r2=r2, pB=pB, s=s, tl=tl: e.tensor_scalar(out=Bb[r2], in0=pB[:, 128:256], scalar1=trT[:, s, 3, tl:tl + 1], scalar2=trT[:, s, 1, tl:tl + 1], op0=ALU.is_equal, op1=ALU.mult),
                             reads=[f"bank{r2}", "trT"], writes=[f"Bb{r2}"])

                    def e_back(t):
                        r2 = t % 4
                        pW = bank(4 + r2)
                        P.op("pe", lambda e, r2=r2, pW=pW: e.matmul(out=pW[:, 0:128], lhsT=Bb[r2], rhs=Ab[r2], start=True, stop=True),
                             reads=[f"Ab{r2}", f"Bb{r2}"], writes=[f"bank{4 + r2}"])
                        if t % 3 == 0:
                            P.op("act", lambda e, r2=r2, pW=pW: e.activation(out=Eb[r2], in_=pW[:, 0:128], func=AF.Copy), reads=[f"bank{4 + r2}", f"Ab{r2}"], writes=[f"Eb{r2}"])
                            P.op("pool", lambda e, r2=r2, t=t: e.tensor_tensor(out=ActT[:, :, t], in0=ActT[:, :, t], in1=Eb[r2], op=ALU.mult), reads=[f"Eb{r2}", "ActT"], writes=[f"WA{t}"])
                        else:
                            P.op("dve", lambda e, pW=pW, t=t: e.tensor_tensor(out=ActT[:, :, t], in0=pW[:, 0:128], in1=ActT[:, :, t], op=ALU.mult), reads=[f"bank{4 + r2}", "ActT"], writes=[f"WA{t}"])

                    for i in range(TP + 2):
                        if i < TP:
                            e_front(i)
                        if 1 <= i <= TP:
                            e_mid(i - 1)
                        if i >= 2:
                            e_back(i - 2)
                    wak = [f"WA{t}" for t in range(TP)]
                    P.mark(f"F{b}_{tp}")
                    for s in range(NSUB):
                        for half in range(128 // GI // 1):
                            pass
                    nV = 0
                    for ig in range(128 // GI):
                        r = ig % NUB
                        P.dma(ubs[r], VB[ig * GI:(ig + 1) * GI].rearrange("i p n -> p i n"), reads=["UBVB"], writes=[f"ubs{r}"])
                        for ii in range(GI):
                            i = ig * GI + ii
                            for s in range(NSUB):
                                for nb2 in range(2):
                                    bk = s * 2 + nb2 if NSUB <= 2 else None
                                    P.op("pe", lambda e, r=r, ii=ii, i=i, s=s, nb2=nb2, bk=bk: e.matmul(out=bank(bk), lhsT=ActT[:, i, s * 128:(s + 1) * 128], rhs=ubs[r][:, ii, nb2 * 512:(nb2 + 1) * 512], start=(i == 0), stop=(i == 127)),
                                         reads=[f"ubs{r}", "ActT"] + (wak if i == 0 else []), writes=[f"bank{bk}"], pe_acc=(i > 0))
                    for s in range(NSUB):
                        for nb2 in range(2):
                            bk = s * 2 + nb2
                            P.op("dve", lambda e, nb2=nb2, bk=bk: e.tensor_tensor(out=ysb[:, nb2 * 512:(nb2 + 1) * 512], in0=bank(bk), in1=gfB[:, nb2 * 512:(nb2 + 1) * 512], op=ALU.mult),
                                 reads=[f"bank{bk}", "gfB"], writes=["ysb"])
                        P.op("dve", lambda e, s=s: e.tensor_tensor(out=ysb, in0=ysb, in1=x1s[:, s, :], op=ALU.add), reads=["ysb", "x1s"], writes=["ysb"])
                        P.op("act", lambda e, s=s: e.activation(out=scr, in_=ysb, func=AF.Square, accum_out=ssq[:, 0:1]), reads=["ysb"], writes=["p2scr", "p2ssq"])
                        P.op("act", lambda e: e.activation(out=rstd[:, 0:1], in_=ssq[:, 0:1], func=AF.Sqrt, scale=1.0 / D, bias=epsb), reads=["p2ssq", "epsb"], writes=["p2rstd"])
                        P.op("dve", lambda e: e.reciprocal(out=rstd[:, 0:1], in_=rstd[:, 0:1]), reads=["p2rstd"], writes=["p2rstd"])
                        P.op("dve", lambda e: e.scalar_tensor_tensor(out=xn, in0=ysb, scalar=rstd[:, 0:1], in1=fingB, op0=ALU.mult, op1=ALU.mult), reads=["ysb", "p2rstd", "fingB"], writes=["pxn"])
                        P.dma(out_d[T0 + s * 128:T0 + (s + 1) * 128, :], xn, reads=["pxn"], writes=["OUT"])
            P.barrier()

        P.barrier()
        P.replay()
    return nc


def make_in_maps(inputs, n_cores, NB, S):
    f = lambda a: np.ascontiguousarray(np.asarray(a, dtype=np.float32))
    x = f(inputs["x"]); c = f(inputs["c"])
    tabs = host_tables(S, f(inputs["rel_bias"]))
    perm = win_perm()
    shared = {
        "ada_w": f(inputs["ada_w"][0]).reshape(8, 128, 6144),
        "ada_bT": f(f(inputs["ada_b"][0]).reshape(48, 128).T),
        "ada_b": f(inputs["ada_b"][0]).reshape(1, 6144),
        "g12T": f(np.stack([f(inputs["norm1_g"][0]).reshape(8, 128).T, f(inputs["norm2_g"][0]).reshape(8, 128).T], axis=1)),
        "w_in": f(f(inputs["w_in"][0])[:, perm]).reshape(8, 128, 2840),
        "w_out": f(inputs["w_out"][0]).reshape(8, 128, 1024),
        "posT": f(f(inputs["cmp_pos"][0]).transpose(2, 0, 1)),
        "w1r": f(f(inputs["cmp_w1"][0]).reshape(2, 32, 64, 256).transpose(0, 2, 1, 3)),
        "w2r": f(f(inputs["cmp_w2"][0]).reshape(2, 2, 128, 64).transpose(2, 0, 1, 3)),
        "lamv": f(np.stack([inputs["lam_q1"][0], inputs["lam_k1"][0], inputs["lam_q2"][0], inputs["lam_k2"][0]])),
        "subg": f(inputs["diff_subln_g"][0]).reshape(1, 128),
        "wq": f(inputs["peer_wq"][0]).reshape(8, 128, 2048),
        "skT": f(f(inputs["peer_sub_keys"][0]).transpose(2, 0, 1)),
        "uT": f(f(inputs["peer_u"][0]).reshape(128, 128, 8, 128).transpose(0, 3, 2, 1)),
        "v": f(inputs["peer_v"][0]).reshape(128, 128, 1024),
        "fing": f(inputs["final_g"]).reshape(1, 1024),
    }
    shared.update(tabs)
    maps = []
    for i in range(n_cores):
        xs = x[i * NB:(i + 1) * NB].reshape(NB * S, D)
        cs = c[i * NB:(i + 1) * NB]
        cT = f(cs.reshape(NB, 8, 128).transpose(2, 1, 0))
        m = dict(shared)
        m["x"] = f(xs)
        m["cT"] = cT
        maps.append(m)
    return maps


def kernel(**inputs):
    B, S, _ = inputs["x"].shape
    n_cores = 8
    NB = B // n_cores
    nc = build(NB, S)
    in_maps = make_in_maps(inputs, n_cores, NB, S)
    res = run_bass_kernel_spmd(nc, in_maps, core_ids=list(range(n_cores)))
    outs = [np.asarray(r["out"]).reshape(NB, S, D) for r in res.results]
    return np.concatenate(outs, axis=0).astype(np.float32)
```
